# Optimizing a Trainium2 kernel written in Bass

```python
import jax, jax.numpy as jnp
from jax import lax
import numpy as np

D_MODEL = 1024
BATCH = 8
SEQ = 8192
DEPTH = 2
DEC_BATCH = 32
DEC_SEQ = 2048
PAST_LEN = 128

PLE_DIM = 256
CHUNK = 128
A_WIDTH = 512
A_GROUPS = 4
B_WIDTH = 512
CONV_WIDTH = 3
C_WIDTH = 512
C_GROUPS = 4
C_GROUP_WIDTH = C_WIDTH // C_GROUPS
POOL_WINDOWS = (2, 4, 8, 16)
ATT_CONFIGS = ((128, 1), (512, 4), (2048, 16))
ATT_HEADS = 4
HEAD_DIM = 128
D_WIDTH = ATT_HEADS * HEAD_DIM
N_BRANCH = 4
BRANCH_WIDTH = 512
N_IN = 2 * A_WIDTH + 3 * B_WIDTH + C_WIDTH + 3 * len(ATT_CONFIGS) * D_WIDTH
N_EXPERTS = 32
TOP_K = 4
D_FF = 1024
SWIGLU_LIMIT = 7.0
SWIGLU_ALPHA = 1.702
EXPERT_BLOCK = 256
ALPHA = (2 * DEPTH) ** 0.25
BETA = (8 * DEPTH) ** -0.25
LN_EPS = 1e-5
NEG_INF = -1e30

kernel_name = 'hybrid_bidir_encoder_two_groups'


def layer_norm(x, gain=None, bias=None):
    xf = x.astype(jnp.float32)
    xc = xf - jnp.mean(xf, axis=-1, keepdims=True)
    y = xc * lax.rsqrt(jnp.mean(xc * xc, axis=-1, keepdims=True) + LN_EPS)
    if gain is not None:
        y = y * gain.astype(jnp.float32) + bias.astype(jnp.float32)
    return y.astype(x.dtype)


def split_points():
    sizes = [A_WIDTH, A_WIDTH, B_WIDTH, B_WIDTH, B_WIDTH, C_WIDTH] + [D_WIDTH] * (3 * len(ATT_CONFIGS))
    return [int(o) for o in np.cumsum(sizes)[:-1]]


def alibi_slopes():
    n = len(ATT_CONFIGS) * ATT_HEADS
    slopes = 2.0 ** (-8.0 * np.arange(1, n + 1) / n)
    return jnp.asarray(slopes, dtype=jnp.float32).reshape(len(ATT_CONFIGS), ATT_HEADS)


def spatial_gating(u, v, w_s, b_s):
    bn, s, _ = v.shape
    vn = layer_norm(v).reshape(bn, s // CHUNK, CHUNK, A_GROUPS, A_WIDTH // A_GROUPS)
    mixed = jnp.einsum('gts,bnsgc->bntgc', w_s, vn) + b_s.T[None, None, :, :, None]
    return u * mixed.reshape(bn, s, A_WIDTH)


def gated_short_conv(h, gate_b, gate_c, conv_w):
    z = gate_c * h
    zp = jnp.pad(z, ((0, 0), (1, 1), (0, 0)))
    conv = conv_w[0] * zp[:, :-2] + conv_w[1] * zp[:, 1:-1] + conv_w[2] * zp[:, 2:]
    return gate_b * conv


def multiscale_pool(z, pool_w, pool_scale):
    bn, s, _ = z.shape
    zf = z.astype(jnp.float32)
    cs = jnp.pad(jnp.cumsum(zf, axis=1), ((0, 0), (1, 0), (0, 0)))
    csg = cs.reshape(bn, s + 1, C_GROUPS, C_GROUP_WIDTH)
    zg = zf.reshape(bn, s, C_GROUPS, C_GROUP_WIDTH)
    t = np.arange(s)
    outs = []
    for g, w in enumerate(POOL_WINDOWS):
        lo = np.clip(t - w // 2, 0, s)
        hi = np.clip(t + w // 2, 0, s)
        cs_g = csg[:, :, g]
        window_sum = jnp.take(cs_g, hi, axis=1) - jnp.take(cs_g, lo, axis=1)
        count = jnp.asarray((hi - lo), dtype=jnp.float32)[None, :, None]
        outs.append(window_sum / count - zg[:, :, g])
    pooled = jnp.stack(outs, axis=2).astype(z.dtype)
    y = jnp.einsum('bsgc,gcd->bsgd', pooled, pool_w)
    return y.reshape(bn, s, C_WIDTH) * pool_scale


def dilated_window_attention(q, k, v, slopes, dilation, radius):
    bn, s, nh, hd = q.shape
    L = s // dilation
    n = bn * dilation
    nb = -(-L // radius)
    Lp = nb * radius

    def to_sub(t):
        return t.reshape(bn, L, dilation, nh, hd).transpose(0, 2, 1, 3, 4).reshape(n, L, nh, hd)

    def band(t):
        tb = jnp.pad(t, ((0, 0), (radius, Lp - L + radius), (0, 0), (0, 0))).reshape(n, nb + 2, radius, nh, hd)
        return jnp.concatenate([tb[:, :-2], tb[:, 1:-1], tb[:, 2:]], axis=2)

    qs, ks, vs = to_sub(q), to_sub(k), to_sub(v)
    qb = jnp.pad(qs, ((0, 0), (0, Lp - L), (0, 0), (0, 0))).reshape(n, nb, radius, nh, hd)
    kw, vw = band(ks), band(vs)
    rel = np.arange(3 * radius)[None, :] - radius - np.arange(radius)[:, None]
    kabs = np.arange(nb)[:, None] * radius + np.arange(3 * radius)[None, :] - radius
    valid = (np.abs(rel)[None] <= radius) & ((kabs >= 0) & (kabs < L))[:, None, :]
    dist = jnp.asarray(np.abs(rel) * dilation, dtype=jnp.float32)
    bias = -slopes.astype(jnp.float32)[:, None, None] * dist[None]
    scores = jnp.einsum('nbqhd,nbkhd->nbhqk', qb, kw, preferred_element_type=jnp.float32) * (hd ** -0.5)
    scores = jnp.where(jnp.asarray(valid)[None, :, None], scores + bias[None, None], NEG_INF)
    lse = jax.nn.logsumexp(scores, axis=-1)
    prob = jnp.exp(scores - lse[..., None]).astype(v.dtype)
    o = jnp.einsum('nbhqk,nbkhd->nbqhd', prob, vw)

    def from_sub(t):
        t = t.reshape((n, Lp) + t.shape[3:])[:, :L]
        return t.reshape((bn, dilation, L) + t.shape[2:]).swapaxes(1, 2).reshape((bn, s) + t.shape[2:])

    return from_sub(o), from_sub(lse.transpose(0, 1, 3, 2))


def dilated_mixture(qkv):
    slopes = alibi_slopes()
    outs, lses = [], []
    for g, (window, dil) in enumerate(ATT_CONFIGS):
        q, k, v = [t.reshape(t.shape[0], t.shape[1], ATT_HEADS, HEAD_DIM) for t in qkv[3 * g:3 * g + 3]]
        o, l = dilated_window_attention(q, k, v, slopes[g], dil, window // (2 * dil))
        outs.append(o)
        lses.append(l)
    wts = jax.nn.softmax(jnp.stack(lses), axis=0).astype(outs[0].dtype)
    y = jnp.einsum('gbsh,gbshd->bshd', wts, jnp.stack(outs))
    return y.reshape(y.shape[0], y.shape[1], D_WIDTH)


def routed_experts(x2, router_w, router_b, w_gate_up, b_gate_up, w_down, b_down):
    T, _ = x2.shape
    A = T * TOP_K
    M = EXPERT_BLOCK
    NB = -(-(A + N_EXPERTS * (M - 1)) // M)
    logits = jnp.dot(x2, router_w, preferred_element_type=jnp.float32) + router_b.astype(jnp.float32)
    top_val, top_idx = lax.top_k(logits, TOP_K)
    probs = jax.nn.softmax(top_val, axis=-1)
    e_flat = top_idx.reshape(A)
    order = jnp.argsort(e_flat)
    e_sorted = e_flat[order]
    counts = jnp.bincount(e_flat, length=N_EXPERTS)
    padded = (counts + M - 1) // M * M
    start_sorted = jnp.cumsum(counts) - counts
    ends_padded = jnp.cumsum(padded)
    start_padded = ends_padded - padded
    dest = start_padded[e_sorted] + jnp.arange(A) - start_sorted[e_sorted]
    slot_tok = jnp.zeros((NB * M,), jnp.int32).at[dest].set((order // TOP_K).astype(jnp.int32))
    slot_w = jnp.zeros((NB * M,), jnp.float32).at[dest].set(probs.reshape(A)[order])
    block_e = jnp.minimum(jnp.searchsorted(ends_padded, jnp.arange(NB) * M, side='right'), N_EXPERTS - 1)

    def expert_block(args):
        tok, wt, e = args
        h = jnp.dot(x2[tok], w_gate_up[e]) + b_gate_up[e]
        g = jnp.minimum(h[:, :D_FF], SWIGLU_LIMIT)
        u = jnp.clip(h[:, D_FF:], -SWIGLU_LIMIT, SWIGLU_LIMIT)
        act = g * jax.nn.sigmoid(SWIGLU_ALPHA * g) * (u + 1.0)
        return (jnp.dot(act, w_down[e]) + b_down[e]) * wt[:, None].astype(x2.dtype)

    y = lax.map(expert_block, (slot_tok.reshape(NB, M), slot_w.reshape(NB, M), block_e))
    return jax.ops.segment_sum(y.reshape(NB * M, -1), slot_tok, num_segments=T)


def encoder_layer(x, p, w_in, spatial_w, spatial_b, conv_w, pool_w, pool_scale, w_branch, w_gate, w_out,
                  ln1_g, ln1_b, router_w, router_b, w_gate_up, b_gate_up, w_down, b_down,
                  w_ple_gate, w_ple_proj, ln2_g, ln2_b):
    bn, s, d = x.shape
    h = x @ w_in
    parts = jnp.split(h, split_points(), axis=-1)
    a_u, a_v, b_h, b_b, b_c, c_z = parts[:6]
    branches = (
        spatial_gating(a_u, a_v, spatial_w, spatial_b),
        gated_short_conv(b_h, b_b, b_c, conv_w),
        multiscale_pool(c_z, pool_w, pool_scale),
        dilated_mixture(parts[6:]),
    )
    merged = None
    for i, y in enumerate(branches):
        term = jax.nn.sigmoid(x @ w_gate[i]) * (y @ w_branch[i])
        merged = term if merged is None else merged + term
    x = layer_norm(ALPHA * x + merged @ w_out, ln1_g, ln1_b)
    moe = routed_experts(x.reshape(bn * s, d), router_w, router_b, w_gate_up, b_gate_up,
                         w_down, b_down).reshape(bn, s, d)
    ple = jax.nn.sigmoid(x @ w_ple_gate) * (p @ w_ple_proj)
    return layer_norm(ALPHA * x + moe + ple, ln2_g, ln2_b)


def run_trunk(x, p, weights):
    for i in range(DEPTH):
        x = encoder_layer(x, p[i], *[w[i] for w in weights])
    return x


def setup_inputs(seed: int = 0) -> dict:
    key = jax.random.key(seed)
    ks = jax.random.split(key, 26)
    f32 = jnp.float32

    def nrm(k, shape, scale):
        return jax.random.normal(k, shape, f32) * scale

    D, E, F = D_MODEL, N_EXPERTS, D_FF
    return {
        'x_prompt': nrm(ks[0], (BATCH, SEQ, D), 1.0),
        'x_sample': nrm(ks[1], (DEC_BATCH, DEC_SEQ, D), 1.0),
        'p_prompt': nrm(ks[2], (DEPTH, BATCH, SEQ, PLE_DIM), 1.0),
        'p_sample': nrm(ks[3], (DEPTH, DEC_BATCH, DEC_SEQ, PLE_DIM), 1.0),
        'w_in': nrm(ks[4], (DEPTH, D, N_IN), D ** -0.5),
        'spatial_w': nrm(ks[5], (DEPTH, A_GROUPS, CHUNK, CHUNK), CHUNK ** -0.5),
        'spatial_b': 1.0 + nrm(ks[6], (DEPTH, A_GROUPS, CHUNK), 0.02),
        'conv_w': nrm(ks[7], (DEPTH, CONV_WIDTH, B_WIDTH), CONV_WIDTH ** -0.5),
        'pool_w': nrm(ks[8], (DEPTH, C_GROUPS, C_GROUP_WIDTH, C_GROUP_WIDTH), C_GROUP_WIDTH ** -0.5),
        'pool_scale': 1.0 + nrm(ks[9], (DEPTH, C_WIDTH), 0.02),
        'w_branch': nrm(ks[10], (DEPTH, N_BRANCH, BRANCH_WIDTH, D), BRANCH_WIDTH ** -0.5),
        'w_gate': nrm(ks[11], (DEPTH, N_BRANCH, D, D), D ** -0.5),
        'w_out': nrm(ks[12], (DEPTH, D, D), BETA * D ** -0.5),
        'ln1_g': 1.0 + nrm(ks[13], (DEPTH, D), 0.02),
        'ln1_b': nrm(ks[14], (DEPTH, D), 0.02),
        'router_w': nrm(ks[15], (DEPTH, D, E), D ** -0.5),
        'router_b': nrm(ks[16], (DEPTH, E), 0.01),
        'w_gate_up': nrm(ks[17], (DEPTH, E, D, 2 * F), D ** -0.5),
        'b_gate_up': nrm(ks[18], (DEPTH, E, 2 * F), 0.02),
        'w_down': nrm(ks[19], (DEPTH, E, F, D), BETA * F ** -0.5),
        'b_down': nrm(ks[20], (DEPTH, E, D), 0.02),
        'w_ple_gate': nrm(ks[21], (DEPTH, D, D), D ** -0.5),
        'w_ple_proj': nrm(ks[22], (DEPTH, PLE_DIM, D), BETA * PLE_DIM ** -0.5),
        'ln2_g': 1.0 + nrm(ks[23], (DEPTH, D), 0.02),
        'ln2_b': nrm(ks[24], (DEPTH, D), 0.02),
    }


def reference(x_prompt, x_sample, p_prompt, p_sample, w_in, spatial_w, spatial_b, conv_w, pool_w,
              pool_scale, w_branch, w_gate, w_out, ln1_g, ln1_b, router_w, router_b, w_gate_up,
              b_gate_up, w_down, b_down, w_ple_gate, w_ple_proj, ln2_g, ln2_b):
    weights = (w_in, spatial_w, spatial_b, conv_w, pool_w, pool_scale, w_branch, w_gate, w_out,
               ln1_g, ln1_b, router_w, router_b, w_gate_up, b_gate_up, w_down, b_down,
               w_ple_gate, w_ple_proj, ln2_g, ln2_b)
    y_prompt = run_trunk(x_prompt, p_prompt, weights)
    y_sample = run_trunk(x_sample, p_sample, weights)
    return (y_prompt, y_sample)
```

```python
import contextlib
import numpy as np
import concourse.bass as bass
import concourse.mybir as mybir
from concourse.bass_utils import run_bass_kernel_spmd

F32 = mybir.dt.float32
BF16 = mybir.dt.bfloat16
I32 = mybir.dt.int32
AF = mybir.ActivationFunctionType
ALU = mybir.AluOpType
AX = mybir.AxisListType

D = 1024
NIN = 7680
E = 32
FF = 1024
PLE = 256
ALPHA = 4.0 ** 0.25
LN_EPS = 1e-5
ATT = ((128, 1), (512, 4), (2048, 16))
POOLW = (2, 4, 8, 16)
NEG = -1.0e30
SCALE = 128.0 ** -0.5


class Buf:
    def __init__(self, t, name):
        self.t = t
        self.name = name
        self.writers = {}
        self.readers = {}
        self.war = {}
        self.dsem = None
        self.dcnt = 0

    def __getitem__(self, key):
        return self.t[key]


def _merge(dst, src):
    for s, v in src.items():
        if dst.get(s, 0) < v:
            dst[s] = v


class K:
    def __init__(self, nc, stack):
        self.nc = nc
        self.stack = stack
        self.eng = {"pe": nc.tensor, "act": nc.scalar, "dve": nc.vector, "pool": nc.gpsimd, "sp": nc.sync}
        self.esem, self.ecnt, self.seen, self.sems, self.ekey = {}, {}, {}, {}, {}
        self.uid = 0
        for e in self.eng:
            s = stack.enter_context(nc.semaphore("es_" + e))
            self.esem[e] = s
            self.ekey[e] = self._key(s)
            self.ecnt[e] = 0
            self.seen[e] = {}
        self.bufs = []
        self.dpool = {True: [], False: []}
        self.root = stack

    def _key(self, s):
        self.uid += 1
        self.sems[self.uid] = s
        return self.uid

    @contextlib.contextmanager
    def scope(self):
        old = self.stack
        nb = len(self.bufs)
        with contextlib.ExitStack() as sub:
            self.stack = sub
            yield
            self.barrier()
            for b in self.bufs[nb:]:
                if b.dsem is not None:
                    self.dpool[b.dsw].append((b.dsem, b.dkey, b.dcnt))
            del self.bufs[nb:]
        self.stack = old

    def sbuf(self, name, shape, dt):
        self.uid += 1
        t = self.stack.enter_context(self.nc.sbuf_tensor(f"{name}_{self.uid}", list(shape), dt))
        b = Buf(t, name)
        self.bufs.append(b)
        return b

    def psum(self, name, shape, dt):
        t = self.stack.enter_context(self.nc.psum_tensor(name, list(shape), dt))
        b = Buf(t, name)
        self.bufs.append(b)
        return b

    def _dsem(self, b, sw):
        if b.dsem is None:
            pool = self.dpool[sw]
            if pool:
                b.dsem, b.dkey, b.dcnt = pool.pop()
                b.dsw = sw
            else:
                b.dsw = sw
                self.uid += 1
                s = self.root.enter_context(self.nc.semaphore(f"ds{self.uid}"))
                b.dsem = s
                b.dkey = self._key(s)
        assert b.dsw == sw, (b.name, 'mixed SW/HW DMA queues on one buffer')
        return b.dsem

    def _wait(self, e, conds):
        seen = self.seen[e]
        for sid, v in conds.items():
            if seen.get(sid, 0) < v:
                self.eng[e].wait_ge(self.sems[sid], v)
                seen[sid] = v

    def _deps(self, e, reads, writes, fresh):
        conds = {}
        for b in reads:
            _merge(conds, b.writers)
        for b in writes:
            if b in fresh:
                w = {}
                _merge(w, b.readers)
                _merge(w, b.writers)
                b.war = w
                b.writers = {}
                b.readers = {}
            _merge(conds, b.war)
        self._wait(e, conds)

    def op(self, e, fn, reads=(), writes=(), fresh=None, inc=True):
        if fresh is None:
            fresh = writes
        self._deps(e, reads, writes, fresh)
        ins = fn(self.eng[e])
        if inc:
            self.ecnt[e] += 1
            ins.then_inc(self.esem[e], 1)
            val = self.ecnt[e]
        else:
            val = self.ecnt[e] + 1
        c = {self.ekey[e]: val}
        for b in reads:
            _merge(b.readers, c)
        for b in writes:
            _merge(b.writers, c)
        return ins

    def dma(self, q, out, in_, wbuf=None, rbuf=None, fresh=True, extra_reads=(), fn=None):
        reads = list(extra_reads) + ([rbuf] if rbuf is not None else [])
        writes = [wbuf] if wbuf is not None else []
        self._deps(q, reads, writes, writes if fresh else [])
        tb = wbuf if wbuf is not None else (rbuf if rbuf is not None else extra_reads[0])
        s = self._dsem(tb, q == 'pool')
        tb.dcnt += 16
        if fn is None:
            ins = self.eng[q].dma_start(out=out, in_=in_)
        else:
            ins = fn(self.eng[q])
        ins.then_inc(s, 16)
        c = {tb.dkey: tb.dcnt}
        for b in reads:
            _merge(b.readers, c)
        for b in writes:
            _merge(b.writers, c)
        return ins

    def barrier(self):
        conds = {}
        for b in self.bufs:
            _merge(conds, b.writers)
            _merge(conds, b.readers)
            _merge(conds, b.war)
        for e in self.eng:
            if self.ecnt[e] > 0:
                _merge(conds, {self.ekey[e]: self.ecnt[e]})
        for e in self.eng:
            self._wait(e, conds)


class Ring:
    def __init__(self, k, name, n, shape, dt, psum=False):
        mk = k.psum if psum else k.sbuf
        self.b = [mk(f"{name}{i}", shape, dt) for i in range(n)]
        self.i = 0

    def next(self):
        b = self.b[self.i % len(self.b)]
        self.i += 1
        return b


def alibi_slopes():
    n = 12
    return (2.0 ** (-8.0 * np.arange(1, n + 1) / n)).reshape(3, 4)


def att_bias_tables():
    q = np.arange(128)[:, None]
    kk = np.arange(256)[None, :] - 64
    rel = kk - q
    band = np.abs(rel) <= 64
    sl = alibi_slopes()
    out = np.zeros((128, 4, 3, 4, 256), np.float32)
    for var in range(4):
        valid = band.copy()
        if var & 1:
            valid &= (kk >= 0)
        if var & 2:
            valid &= (kk < 128)
        for g, (_, dil) in enumerate(ATT):
            for h in range(4):
                b = -sl[g, h] * np.abs(rel) * dil
                out[:, var, g, h, :] = np.where(valid, b, NEG)
    return out


def pool_corr():
    out = np.ones((2, 4, 8), np.float32)
    Sx = 4096
    for g, w in enumerate(POOLW):
        t = np.arange(8)
        out[0, g] = w / (t + w // 2 - np.maximum(t - w // 2, 0))
        t = Sx - 8 + np.arange(8)
        out[1, g] = w / (np.minimum(t + w // 2, Sx) - (t - w // 2))
    return np.broadcast_to(out[None], (128, 2, 4, 8)).copy()


def pool_invcount(S):
    out = np.zeros((3, 4, 512), np.float32)
    for g, w in enumerate(POOLW):
        out[0, g, :] = 1.0 / w
        t = np.arange(512)
        out[1, g, :] = 1.0 / (np.minimum(t + w // 2, 10 ** 9) - np.maximum(t - w // 2, 0))
        tt = np.arange(512) + 1024
        out[2, g, :] = 1.0 / (np.minimum(tt + w // 2, 1536) - (tt - w // 2))
    return np.broadcast_to(out[None], (128, 3, 4, 512)).copy()


def build(seqs, NL=2, BLK=512, dbg=(), phases=("p1", "pa", "p2a", "p2b", "p3", "p4")):
    T = sum(seqs)
    NT = T // 512
    A = T * 4
    NB = -(-(A + E * (BLK - 1)) // BLK)
    NSLOT = NB * BLK
    nc = bass.Bass("TRN2", target_bir_lowering=False)

    def din(name, shape, dt=F32):
        return nc.dram_tensor(name, list(shape), dt, kind="ExternalInput").ap()

    def dscr(name, shape, dt):
        kind = "ExternalOutput" if name in dbg else "Internal"
        return nc.dram_tensor(name, list(shape), dt, kind=kind).ap()

    x_in = din("x", [T, D])
    p_in = din("p", [NL, T, PLE])
    w_in = din("w_in", [NL, D, NIN])
    spT = din("spT", [NL, 128, 4, 128])
    sbias = din("sbias", [NL, 128, 4, 128])
    convw = din("convw", [NL, 128, 3, 4])
    poolw = din("poolw", [NL, 128, 4, 128])
    pscale = din("pscale", [NL, 128, 4])
    w_branch = din("w_branch", [NL, 4, 512, D])
    w_gate = din("w_gate", [NL, 4, D, D])
    w_out = din("w_out", [NL, D, D])
    ln1g = din("ln1g", [NL, 128, D])
    ln1b = din("ln1b", [NL, 128, D])
    router_w = din("router_w", [NL, D, E])
    router_b = din("router_b", [NL, 128, E])
    w_gu = din("w_gu", [NL * E * D, 2 * FF])
    b_gu = din("b_gu", [NL * E * 128, 16])
    w_dn = din("w_dn", [NL * E * FF, D])
    b_dn = din("b_dn", [NL * E, D])
    w_pg = din("w_pg", [NL, D, D])
    w_pp = din("w_pp", [NL, PLE, D])
    ln2g = din("ln2g", [NL, 128, D])
    ln2b = din("ln2b", [NL, 128, D])
    identf = din("identf", [128, 128])
    abias = din("abias", [128, 4 * 3 * 4 * 256])
    pcorr = din("pcorr", [128, 2 * 4 * 8])
    dbgT = {}
    if "BED" in dbg:
        dbgT["BED"] = nc.dram_tensor("BED", [1, NB], I32, kind="ExternalOutput").ap()
    for nm in ("YA2", "YB", "YC", "YD"):
        if "YD" in dbg:
            dbgT[nm] = nc.dram_tensor(nm, [4, 128, T], BF16, kind="ExternalOutput").ap()

    tril = din("tril", [128, 128])
    y_out = nc.dram_tensor("y", [T, D], F32, kind="ExternalOutput").ap()

    QKV = dscr("QKV", [9, T, 512], BF16)
    YA = dscr("YA", [4, 128, T], BF16)
    BB = dscr("BB", [4, 128, T], BF16)
    ZZ = dscr("ZZ", [4, 128, T], BF16)
    CZ = dscr("CZ", [4, 128, T], BF16)
    OO = dscr("OO", [3, T, 512], BF16)
    LSE = dscr("LSE", [3, T, 4], F32)
    MG = dscr("MG", [8, 128, T], BF16)
    X1 = dscr("X1", [T, D], F32)
    X1B = dscr("X1B", [T, D], BF16)
    X1T = dscr("X1T", [8, 128, T], BF16)
    XS = dscr("XS", [NSLOT, D], BF16)
    YSH = [dscr("YSa", [NSLOT, 512], F32), dscr("YSb", [NSLOT, 512], F32)]
    XN = dscr("XN", [T, D], F32)
    LGD = dscr("LGD", [T, 32], F32)
    DESTD = dscr("DESTD", [T, 4], I32)
    PROBD = dscr("PROBD", [T, 4], F32)

    with contextlib.ExitStack() as st:
        k = K(nc, st)
        ps = [k.psum(f"ps{i}", [128, 512], F32) for i in range(8)]
        psi = [0]

        def nps():
            b = ps[psi[0] % 8]
            psi[0] += 1
            return b

        flip = [0]

        def evq():
            flip[0] += 1
            return "act" if flip[0] % 2 else "dve"

        def copy(e, out, in_, reads, writes, fresh=None):
            if e == "act":
                return k.op("act", lambda g: g.activation(out=out, in_=in_, func=AF.Copy), reads, writes, fresh)
            return k.op(e, lambda g: g.tensor_copy(out=out, in_=in_), reads, writes, fresh)

        identb = k.sbuf("identb", [128, 128], BF16)
        k.dma("pool", identb[:], identf, wbuf=identb)
        identF = k.sbuf("identF", [128, 128], F32)
        k.dma("sp", identF[:], identf, wbuf=identF)

        def phase1(l, xsrc):
            wa = k.sbuf("w1", [128, 8 * NIN], BF16)
            k1_spT = k.sbuf("k1_spT", [128, 4, 128], BF16)
            k1_sbias = k.sbuf("k1_sbias", [128, 4, 128], F32)
            r_xb = Ring(k, "xb", 1, [128, 4, 1024], BF16)
            r_xT = Ring(k, "xT", 2, [128, 8, 512], BF16)
            r_auT = Ring(k, "auT", 2, [128, 4, 512], BF16)
            r_fm = Ring(k, "fm", 4, [128, 4, 512], BF16)
            r_st = Ring(k, "st", 4, [128, 16], F32)
            r_vn = Ring(k, "vn", 2, [128, 512], BF16)
            r_tmp = Ring(k, "tmp", 2, [128, 512], F32)
            r_qkv = Ring(k, "qkvs", 2, [128, 3, 512], BF16)
            w_sb = wa[:, 0:8 * NIN].rearrange("p (c n) -> p c n", c=8)
            for c in range(8):
                for h in range(4):
                    k.dma("pool", w_sb[:, c, h * 1920:(h + 1) * 1920], w_in[l, c * 128:(c + 1) * 128, h * 1920:(h + 1) * 1920],
                          wbuf=wa, fresh=(c == 0 and h == 0))
            spT_sb = k1_spT
            k.dma("pool", spT_sb[:], spT[l], wbuf=spT_sb)
            k.dma("sp", k1_sbias[:], sbias[l], wbuf=k1_sbias)
            for i in range(NT):
                t0 = i * 512
                xb = r_xb.next()
                k.dma("pool", xb[:], xsrc[t0:t0 + 512, :].rearrange("(s p) f -> p s f", p=128), wbuf=xb)
                xT = r_xT.next()
                for s in range(4):
                    pt = nps()
                    ptb = pt[:].bitcast(BF16)
                    for c in range(8):
                        k.op("pe", lambda g: g.transpose(out=ptb[:, c * 128:(c + 1) * 128], in_=xb[:, s, c * 128:(c + 1) * 128],
                                                         identity=identb[:]),
                             [xb, identb], [pt], [pt] if c == 0 else [], inc=(c == 7))
                    copy(evq(), xT[:, :, s * 128:(s + 1) * 128], ptb.rearrange("p (c t) -> p c t", c=8), [pt], [xT],
                         [xT] if s == 0 else [])

                def mm_fm(fb):
                    pm = nps()
                    for c in range(8):
                        k.op("pe", lambda g: g.matmul(pm[:], lhsT=w_sb[:, c, fb * 128:(fb + 1) * 128], rhs=xT[:, c, :],
                                                      start=(c == 0), stop=(c == 7)),
                             [wa, xT], [pm], [pm] if c == 0 else [], inc=(c == 7))
                    return pm

                def mm_tm(s, col0):
                    pm = nps()
                    for c in range(8):
                        k.op("pe", lambda g: g.matmul(pm[:], lhsT=xT[:, c, s * 128:(s + 1) * 128], rhs=w_sb[:, c, col0:col0 + 512],
                                                      start=(c == 0), stop=(c == 7)),
                             [wa, xT], [pm], [pm] if c == 0 else [], inc=(c == 7))
                    return pm

                auT = r_auT.next()
                for fb in range(4):
                    pm = mm_fm(fb)
                    copy("act", auT[:, fb, :], pm[:], [pm], [auT], [auT] if fb == 0 else [])
                yA = r_fm.next()
                for s in range(4):
                    pv = mm_tm(s, 512)
                    stt = r_st.next()
                    k.op("dve", lambda g: g.bn_stats(out=stt[:, 0:6], in_=pv[:]), [pv], [stt])
                    k.op("dve", lambda g: g.bn_aggr(out=stt[:, 6:8], in_=stt[:, 0:6]), [stt], [stt], [])
                    k.op("act", lambda g: g.activation(out=stt[:, 8:9], in_=stt[:, 7:8], func=AF.Sqrt, bias=k1_eps[:, 0:1], scale=1.0),
                         [stt, k1_eps], [stt], [])
                    k.op("dve", lambda g: g.reciprocal(out=stt[:, 9:10], in_=stt[:, 8:9]), [stt], [stt], [])
                    vn = r_vn.next()
                    k.op("dve", lambda g: g.tensor_scalar(out=vn[:], in0=pv[:], scalar1=stt[:, 6:7], scalar2=stt[:, 9:10],
                                                          op0=ALU.subtract, op1=ALU.mult), [pv, stt], [vn])
                    pm = nps()
                    for gq in range(4):
                        k.op("pe", lambda g: g.matmul(pm[:, gq * 128:(gq + 1) * 128], lhsT=vn[:, gq * 128:(gq + 1) * 128],
                                                      rhs=spT_sb[:, gq, :], start=True, stop=True),
                             [vn, spT_sb], [pm], [pm] if gq == 0 else [], inc=(gq == 3))
                    tmp = r_tmp.next()
                    k.op("dve", lambda g: g.tensor_tensor(out=tmp[:], in0=pm[:], in1=k1_sbias[:].rearrange("p g t -> p (g t)"),
                                                          op=ALU.add), [pm, k1_sbias], [tmp])
                    k.op("pool", lambda g: g.tensor_tensor(out=yA[:, :, s * 128:(s + 1) * 128], in0=auT[:, :, s * 128:(s + 1) * 128],
                                                           in1=tmp[:].rearrange("p (g t) -> p g t", g=4), op=ALU.mult),
                         [auT, tmp], [yA], [yA] if s == 0 else [])
                k.dma("sp", YA[:, :, t0:t0 + 512].rearrange("g p t -> p g t"), yA[:], rbuf=yA)
                bh = r_auT.next()
                for j in range(4):
                    pm = mm_fm(8 + j)
                    copy("act", bh[:, j, :], pm[:], [pm], [bh], [bh] if j == 0 else [])
                zz = r_fm.next()
                for j in range(4):
                    pm = mm_fm(16 + j)
                    k.op("dve", lambda g: g.tensor_tensor(out=zz[:, j, :], in0=pm[:], in1=bh[:, j, :], op=ALU.mult),
                         [pm, bh], [zz], [zz] if j == 0 else [])
                k.dma("sp", ZZ[:, :, t0:t0 + 512].rearrange("g p t -> p g t"), zz[:], rbuf=zz)
                bb = r_fm.next()
                for j in range(4):
                    pm = mm_fm(12 + j)
                    copy(evq(), bb[:, j, :], pm[:], [pm], [bb], [bb] if j == 0 else [])
                k.dma("sp", BB[:, :, t0:t0 + 512].rearrange("g p t -> p g t"), bb[:], rbuf=bb)
                cz = r_fm.next()
                for j in range(4):
                    pm = mm_fm(20 + j)
                    copy(evq(), cz[:, j, :], pm[:], [pm], [cz], [cz] if j == 0 else [])
                k.dma("sp", CZ[:, :, t0:t0 + 512].rearrange("g p t -> p g t"), cz[:], rbuf=cz)
                for s in range(4):
                    r0 = t0 + s * 128
                    for j3 in range(3):
                        qs = r_qkv.next()
                        for jj in range(3):
                            pm = mm_tm(s, 3072 + (j3 * 3 + jj) * 512)
                            copy(evq(), qs[:, jj, :], pm[:], [pm], [qs], [qs] if jj == 0 else [])
                        k.dma("sp", QKV[j3 * 3:j3 * 3 + 3, r0:r0 + 128, :].rearrange("j t f -> t j f"), qs[:], rbuf=qs)

        def phaseA(l):
            ab = k.sbuf("abias", [128, 48 * 256], F32)
            for q4 in range(4):
                k.dma("sp", ab[:, q4 * 3072:(q4 + 1) * 3072], abias[:, q4 * 3072:(q4 + 1) * 3072], wbuf=ab, fresh=(q4 == 0))
            r_q = Ring(k, "aq", 2, [128, 8, 512], BF16)
            r_k = Ring(k, "ak", 2, [128, 9, 512], BF16)
            r_v = Ring(k, "av", 2, [128, 9, 512], BF16)
            r_qT = Ring(k, "aqT", 2, [128, 4, 1024], BF16)
            r_kT = Ring(k, "akT", 2, [128, 4, 1152], BF16)
            r_sb = Ring(k, "asb", 4, [128, 4, 256], F32)
            r_p = Ring(k, "ap", 4, [128, 4, 256], BF16)
            r_pt = Ring(k, "apt", 4, [128, 4, 2, 128], BF16)
            r_st = Ring(k, "ast", 8, [128, 32], F32)
            r_o = Ring(k, "ao", 2, [128, 8, 512], BF16)
            r_l = Ring(k, "al", 2, [128, 8, 4], F32)
            pS = [(ps[0], ps[1]), (ps[2], ps[3])]
            pT = [ps[4], ps[5]]
            pU = [ps[6], ps[7]]
            cnt = [0]

            def unit(seq_off, S, g, d, r, a, b):
                L = S // d
                ntile = L // 128
                nq = b - a

                def sub(t2d):
                    return t2d[seq_off:seq_off + S, :].rearrange("(i d) f -> d i f", d=d)[r]
                qd, kd, vd = sub(QKV[3 * g]), sub(QKV[3 * g + 1]), sub(QKV[3 * g + 2])
                qb = r_q.next()
                k.dma("sp", qb[:, 0:nq, :], qd[128 * a:128 * b, :].rearrange("(m p) f -> p m f", p=128), wbuf=qb)
                kb, vb = r_k.next(), r_v.next()
                for (buf, src, q) in ((kb, kd, "sp"), (vb, vd, "act")):
                    first = True
                    m_lo, m_hi = max(a, 1), min(b, ntile - 1)
                    if m_lo <= m_hi:
                        k.dma(q, buf[:, m_lo - a:m_hi - a + 1, :],
                              src[128 * m_lo - 64:128 * m_hi + 64, :].rearrange("(m p) f -> p m f", p=128), wbuf=buf, fresh=first)
                        first = False
                    if a == 0:
                        k.dma(q, buf[64:128, 0, :], src[0:64, :], wbuf=buf, fresh=first)
                        first = False
                        k.op("pool", lambda e: e.memset(buf[0:64, 0, :], 0.0), [], [buf], [])
                    if b == ntile:
                        k.dma(q, buf[0:64, ntile - a, :], src[L - 64:L, :], wbuf=buf, fresh=first)
                        first = False
                        k.op("pool", lambda e: e.memset(buf[64:128, ntile - a, :], 0.0), [], [buf], [])
                qT, kT = r_qT.next(), r_kT.next()
                for (src, dst, n) in ((qb, qT, nq), (kb, kT, nq + 1)):
                    j = 0
                    while j < n:
                        w = min(2, n - j)
                        pt = pT[cnt[0] % 2]
                        cnt[0] += 1
                        ptb = pt[:].bitcast(BF16)
                        for h in range(4):
                            for jj in range(w):
                                last = (h == 3 and jj == w - 1)
                                k.op("pe", lambda e: e.transpose(out=ptb[:, (h * w + jj) * 128:(h * w + jj + 1) * 128],
                                                                 in_=src[:, j + jj, h * 128:(h + 1) * 128], identity=identb[:]),
                                     [src, identb], [pt], [pt] if (h == 0 and jj == 0) else [], inc=last)
                        copy(evq(), dst[:, :, j * 128:(j + w) * 128], ptb[:, 0:4 * w * 128].rearrange("p (h x) -> p h x", h=4),
                             [pt], [dst], [dst] if j == 0 else [])
                        j += w
                ost, lst = r_o.next(), r_l.next()

                def stageA(j):
                    pa_, pb_ = pS[j % 2]
                    for h in range(4):
                        pp = pa_ if h < 2 else pb_
                        hh = h % 2
                        k.op("pe", lambda e: e.matmul(pp[:, hh * 256:(hh + 1) * 256], lhsT=qT[:, h, j * 128:(j + 1) * 128],
                                                      rhs=kT[:, h, j * 128:j * 128 + 256], start=True, stop=True),
                             [qT, kT], [pp], [pp] if hh == 0 else [], inc=(hh == 1))

                stt_ = {}
                sb_ = {}
                pb_d = {}
                ptu = {}

                def stageB1(j):
                    ja = a + j
                    var = (1 if ja == 0 else 0) | (2 if ja == ntile - 1 else 0)
                    pa_, pb_ = pS[j % 2]
                    sb = r_sb.next()
                    sb_[j] = sb
                    for half, pp in enumerate((pa_, pb_)):
                        base = ((var * 3 + g) * 4 + half * 2) * 256
                        k.op("dve", lambda e: e.scalar_tensor_tensor(out=sb[:, half * 2:half * 2 + 2, :].rearrange("p h x -> p (h x)"),
                                                                     in0=pp[:], scalar=SCALE, in1=ab[:, base:base + 512],
                                                                     op0=ALU.mult, op1=ALU.add),
                             [pp, ab], [sb], [sb] if half == 0 else [])
                    stt = r_st.next()
                    stt_[j] = stt
                    k.op("dve", lambda e: e.tensor_reduce(out=stt[:, 0:4], in_=sb[:], axis=AX.X, op=ALU.max), [sb], [stt])
                    k.op("dve", lambda e: e.tensor_scalar(out=stt[:, 4:8], in0=stt[:, 0:4], scalar1=-1.0, scalar2=None, op0=ALU.mult),
                         [stt], [stt], [])

                def stageB2(j):
                    sb, stt = sb_[j], stt_[j]
                    pb = r_p.next()
                    pb_d[j] = pb
                    for h in range(4):
                        k.op("act", lambda e: e.activation(out=pb[:, h, :], in_=sb[:, h, :], func=AF.Exp, bias=stt[:, 4 + h:5 + h],
                                                           scale=1.0, accum_out=stt[:, 8 + h:9 + h]),
                             [sb, stt], [pb, stt], [pb] if h == 0 else [])

                def stageB3(j):
                    pb = pb_d[j]
                    pt = pT[cnt[0] % 2]
                    pu = pU[cnt[0] % 2]
                    cnt[0] += 1
                    ptb = pt[:].bitcast(BF16)
                    for h in range(4):
                        for c in range(2):
                            k.op("pe", lambda e: e.transpose(out=ptb[:, (h * 2 + c) * 128:(h * 2 + c + 1) * 128],
                                                             in_=pb[:, h, c * 128:(c + 1) * 128], identity=identb[:]),
                                 [pb, identb], [pt], [pt] if (h == 0 and c == 0) else [], inc=(h == 3 and c == 1))
                    ptt = r_pt.next()
                    copy("act", ptt[:].rearrange("p h c t -> p (h c t)"), ptb, [pt], [ptt])
                    ptu[j] = (ptt, pu)

                def stagePV(j):
                    ptt, pu = ptu[j]
                    for h in range(4):
                        for c in range(2):
                            k.op("pe", lambda e: e.matmul(pu[:, h * 128:(h + 1) * 128], lhsT=ptt[:, h, c, :],
                                                          rhs=vb[:, j + c, h * 128:(h + 1) * 128], start=(c == 0), stop=(c == 1)),
                                 [ptt, vb], [pu], [pu] if (h == 0 and c == 0) else [], inc=(h == 3 and c == 1))

                def tail1(j):
                    stt = stt_[j]
                    k.op("dve", lambda e: e.reciprocal(out=stt[:, 12:16], in_=stt[:, 8:12]), [stt], [stt], [])

                def tail2(j):
                    stt = stt_[j]
                    ptt, pu = ptu[j]
                    for h in range(4):
                        k.op("act", lambda e: e.activation(out=ost[:, j, h * 128:(h + 1) * 128], in_=pu[:, h * 128:(h + 1) * 128],
                                                           func=AF.Copy, scale=stt[:, 12 + h:13 + h]),
                             [pu, stt], [ost], [ost] if (j == 0 and h == 0) else [])
                    k.op("act", lambda e: e.activation(out=stt[:, 16:20], in_=stt[:, 8:12], func=AF.Ln), [stt], [stt], [])

                def tail3(j):
                    stt = stt_[j]
                    k.op("dve", lambda e: e.tensor_tensor(out=lst[:, j, :], in0=stt[:, 16:20], in1=stt[:, 0:4], op=ALU.add),
                         [stt], [lst], [lst] if j == 0 else [])

                pairs = [list(range(j, min(j + 2, nq))) for j in range(0, nq, 2)]
                for j in pairs[0]:
                    stageA(j)
                prev = None
                for pi, pr in enumerate(pairs):
                    for j in pr:
                        stageB1(j)
                    if prev is not None:
                        for j in prev:
                            tail1(j)
                        for j in prev:
                            tail2(j)
                        for j in prev:
                            tail3(j)
                    for j in pr:
                        stageB2(j)
                    if pi + 1 < len(pairs):
                        for j in pairs[pi + 1]:
                            stageA(j)
                    for j in pr:
                        stageB3(j)
                    for j in pr:
                        stagePV(j)
                    prev = pr
                for j in prev:
                    tail1(j)
                for j in prev:
                    tail2(j)
                for j in prev:
                    tail3(j)
                od = sub(OO[g])
                k.dma("sp", od[128 * a:128 * b, :].rearrange("(m p) f -> p m f", p=128), ost[:, 0:nq, :], rbuf=ost)
                ld = sub(LSE[g])
                k.dma("act", ld[128 * a:128 * b, :].rearrange("(m p) f -> p m f", p=128), lst[:, 0:nq, :], rbuf=lst)

            off = 0
            for S in seqs:
                for g, (_, d) in enumerate(ATT):
                    L = S // d
                    ntile = L // 128
                    for r in range(d):
                        for a in range(0, ntile, 8):
                            unit(off, S, g, d, r, a, min(a + 8, ntile))
                off += S


        def load_xT(xsrc, t0, r_xb, r_xT):
            xb = r_xb.next()
            k.dma("pool", xb[:], xsrc[t0:t0 + 512, :].rearrange("(s p) f -> p s f", p=128), wbuf=xb)
            xT = r_xT.next()
            for s in range(4):
                pt = nps()
                ptb = pt[:].bitcast(BF16)
                for c in range(8):
                    k.op("pe", lambda g: g.transpose(out=ptb[:, c * 128:(c + 1) * 128], in_=xb[:, s, c * 128:(c + 1) * 128],
                                                     identity=identb[:]),
                         [xb, identb], [pt], [pt] if c == 0 else [], inc=(c == 7))
                copy(evq(), xT[:, :, s * 128:(s + 1) * 128], ptb.rearrange("p (c t) -> p c t", c=8), [pt], [xT],
                     [xT] if s == 0 else [])
            return xT

        def seq_pos():
            off = 0
            for S in seqs:
                for i in range(S // 512):
                    yield off + i * 512, i == 0, i == S // 512 - 1
                off += S

        def phase2a(l, xsrc):
            wg = k.sbuf("wg", [128, 4, 8, 1024], BF16)
            wbr = k.sbuf("wbr", [128, 4, 4, 1024], BF16)
            for i in range(4):
                for c in range(8):
                    k.dma("pool", wg[:, i, c, :], w_gate[l, i, c * 128:(c + 1) * 128, :], wbuf=wg, fresh=(i == 0 and c == 0))
                for c in range(4):
                    k.dma("pool", wbr[:, i, c, :], w_branch[l, i, c * 128:(c + 1) * 128, :], wbuf=wbr, fresh=(i == 0 and c == 0))
            cw = k.sbuf("cw", [128, 3, 4], F32)
            k.dma("sp", cw[:], convw[l], wbuf=cw)
            pw = k.sbuf("pw", [128, 4, 128], BF16)
            k.dma("pool", pw[:], poolw[l], wbuf=pw)
            psc = k.sbuf("psc", [128, 4], F32)
            k.dma("sp", psc[:], pscale[l], wbuf=psc)
            pc = k.sbuf("pcorr", [128, 2, 4, 8], F32)
            k.dma("sp", pc[:].rearrange("p a g t -> p (a g t)"), pcorr, wbuf=pc)
            r_xb = Ring(k, "xb", 1, [128, 4, 1024], BF16)
            r_xT = Ring(k, "xT", 1, [128, 8, 512], BF16)
            r_y = [Ring(k, f"y{i}", 1, [128, 4, 512], BF16) for i in range(4)]
            r_bb = Ring(k, "bb", 1, [128, 4, 512], BF16)
            r_zh = Ring(k, "zh", 1, [128, 4, 514], BF16)
            r_czh = Ring(k, "czh", 1, [128, 4, 528], BF16)
            r_og = Ring(k, "og", 2, [128, 3, 512], BF16)
            r_ls = Ring(k, "ls", 2, [128, 4, 3, 4], F32)
            r_t = Ring(k, "t2", 5, [128, 528], F32)
            r_pl = Ring(k, "pl", 2, [128, 512], BF16)
            r_sm = Ring(k, "sm", 4, [128, 64], F32)
            r_acc = Ring(k, "acc", 2, [128, 512], F32)
            r_ydt = Ring(k, "ydt", 2, [128, 512], BF16)
            r_sg = Ring(k, "sg", 3, [128, 512], BF16)
            r_ma = Ring(k, "ma", 2, [128, 512], F32)
            r_mt = Ring(k, "mt", 2, [128, 512], F32)
            r_mg = Ring(k, "mg", 3, [128, 512], BF16)
            for (t0, first, last) in seq_pos():
                xT = load_xT(xsrc, t0, r_xb, r_xT)
                yA = r_y[0].next()
                k.dma("sp", yA[:], YA[:, :, t0:t0 + 512].rearrange("g p t -> p g t"), wbuf=yA)
                bb = r_bb.next()
                k.dma("sp", bb[:], BB[:, :, t0:t0 + 512].rearrange("g p t -> p g t"), wbuf=bb)
                zh = r_zh.next()
                lo, hi = (1 if first else 0), (513 if last else 514)
                k.dma("act", zh[:, :, lo:hi], ZZ[:, :, t0 - 1 + lo:t0 - 1 + hi].rearrange("g p t -> p g t"), wbuf=zh)
                if first:
                    k.op("pool", lambda e: e.memset(zh[:, :, 0:1], 0.0), [], [zh], [])
                if last:
                    k.op("pool", lambda e: e.memset(zh[:, :, 513:514], 0.0), [], [zh], [])
                czh = r_czh.next()
                lo, hi = (8 if first else 0), (520 if last else 528)
                k.dma("act", czh[:, :, lo:hi], CZ[:, :, t0 - 8 + lo:t0 - 8 + hi].rearrange("g p t -> p g t"), wbuf=czh)
                if first:
                    k.op("pool", lambda e: e.memset(czh[:, :, 0:8], 0.0), [], [czh], [])
                if last:
                    k.op("pool", lambda e: e.memset(czh[:, :, 520:528], 0.0), [], [czh], [])
                ls = r_ls.next()
                for g in range(3):
                    k.dma("act", ls[:, :, g, :], LSE[g, t0:t0 + 512, :].rearrange("(s p) h -> p s h", p=128), wbuf=ls, fresh=(g == 0))
                yB = r_y[1].next()
                for fb in range(4):
                    tb = r_t.next()
                    k.op("dve", lambda e: e.tensor_scalar(out=tb[:, 0:512], in0=zh[:, fb, 0:512], scalar1=cw[:, 0, fb:fb + 1], scalar2=None,
                                                          op0=ALU.mult), [zh, cw], [tb])
                    for j in (1, 2):
                        k.op("dve", lambda e: e.scalar_tensor_tensor(out=tb[:, 0:512], in0=zh[:, fb, j:j + 512], scalar=cw[:, j, fb:fb + 1],
                                                                     in1=tb[:, 0:512], op0=ALU.mult, op1=ALU.add), [zh, cw, tb], [tb], [])
                    k.op("dve", lambda e: e.tensor_tensor(out=yB[:, fb, :], in0=tb[:, 0:512], in1=bb[:, fb, :], op=ALU.mult),
                         [tb, bb], [yB], [yB] if fb == 0 else [])
                yC = r_y[2].next()
                for g in range(4):
                    w = POOLW[g]
                    cur = r_t.next()
                    k.op("pool", lambda e: e.tensor_tensor(out=cur[:, 1:528], in0=czh[:, g, 0:527], in1=czh[:, g, 1:528], op=ALU.add),
                         [czh], [cur])
                    half = 1
                    lo_, hi_ = 1, 528
                    while half * 2 < w:
                        nxt = r_t.next()
                        nlo, nhi = lo_ + half, hi_ - half
                        k.op("pool", lambda e: e.tensor_tensor(out=nxt[:, nlo:nhi], in0=cur[:, nlo - half:nhi - half],
                                                               in1=cur[:, nlo + half:nhi + half], op=ALU.add), [cur], [nxt])
                        cur, lo_, hi_, half = nxt, nlo, nhi, half * 2
                    if first:
                        k.op("dve", lambda e: e.tensor_tensor(out=cur[:, 8:16], in0=cur[:, 8:16], in1=pc[:, 0, g, :], op=ALU.mult),
                             [cur, pc], [cur], [])
                    if last:
                        k.op("dve", lambda e: e.tensor_tensor(out=cur[:, 512:520], in0=cur[:, 512:520], in1=pc[:, 1, g, :], op=ALU.mult),
                             [cur, pc], [cur], [])
                    pl = r_pl.next()
                    k.op("dve", lambda e: e.scalar_tensor_tensor(out=pl[:], in0=cur[:, 8:520], scalar=1.0 / w, in1=czh[:, g, 8:520],
                                                                 op0=ALU.mult, op1=ALU.subtract), [cur, czh], [pl])
                    pm = nps()
                    k.op("pe", lambda e: e.matmul(pm[:], lhsT=pw[:, g, :], rhs=pl[:], start=True, stop=True), [pw, pl], [pm])
                    k.op("act", lambda e: e.activation(out=yC[:, g, :], in_=pm[:], func=AF.Copy, scale=psc[:, g:g + 1]),
                         [pm, psc], [yC], [yC] if g == 0 else [])
                yD = r_y[3].next()
                for s in range(4):
                    og = r_og.next()
                    k.dma("sp", og[:], OO[:, t0 + s * 128:t0 + (s + 1) * 128, :].rearrange("g p f -> p g f"), wbuf=og)
                    sm = r_sm.next()
                    lsv = ls[:, s, :, :]
                    k.op("dve", lambda e: e.tensor_tensor(out=sm[:, 0:4], in0=lsv[:, 0, :], in1=lsv[:, 1, :], op=ALU.max), [ls], [sm])
                    k.op("dve", lambda e: e.tensor_tensor(out=sm[:, 0:4], in0=sm[:, 0:4], in1=lsv[:, 2, :], op=ALU.max), [ls, sm], [sm], [])
                    for g in range(3):
                        k.op("dve", lambda e: e.tensor_tensor(out=sm[:, 4 + 4 * g:8 + 4 * g], in0=lsv[:, g, :], in1=sm[:, 0:4],
                                                              op=ALU.subtract), [ls, sm], [sm], [])
                    k.op("act", lambda e: e.activation(out=sm[:, 16:28], in_=sm[:, 4:16], func=AF.Exp), [sm], [sm], [])
                    k.op("dve", lambda e: e.tensor_tensor(out=sm[:, 28:32], in0=sm[:, 16:20], in1=sm[:, 20:24], op=ALU.add), [sm], [sm], [])
                    k.op("dve", lambda e: e.tensor_tensor(out=sm[:, 28:32], in0=sm[:, 28:32], in1=sm[:, 24:28], op=ALU.add), [sm], [sm], [])
                    k.op("dve", lambda e: e.reciprocal(out=sm[:, 32:36], in_=sm[:, 28:32]), [sm], [sm], [])
                    for g in range(3):
                        k.op("dve", lambda e: e.tensor_tensor(out=sm[:, 36 + 4 * g:40 + 4 * g], in0=sm[:, 16 + 4 * g:20 + 4 * g],
                                                              in1=sm[:, 32:36], op=ALU.mult), [sm], [sm], [])
                    acc = r_acc.next()
                    for h in range(4):
                        hs = slice(h * 128, (h + 1) * 128)
                        k.op("dve", lambda e: e.tensor_scalar(out=acc[:, hs], in0=og[:, 0, hs], scalar1=sm[:, 36 + h:37 + h], scalar2=None,
                                                              op0=ALU.mult), [og, sm], [acc], [acc] if h == 0 else [])
                        for g in (1, 2):
                            last_ = (g == 2)
                            k.op("dve", lambda e: e.scalar_tensor_tensor(out=acc[:, hs], in0=og[:, g, hs],
                                                                         scalar=sm[:, 36 + 4 * g + h:37 + 4 * g + h], in1=acc[:, hs],
                                                                         op0=ALU.mult, op1=ALU.add), [og, sm, acc], [acc], [])
                    ydt = r_ydt.next()
                    copy("act", ydt[:], acc[:], [acc], [ydt])
                    pt = nps()
                    ptb = pt[:].bitcast(BF16)
                    for h in range(4):
                        k.op("pe", lambda e: e.transpose(out=ptb[:, h * 128:(h + 1) * 128], in_=ydt[:, h * 128:(h + 1) * 128], identity=identb[:]),
                             [ydt, identb], [pt], [pt] if h == 0 else [], inc=(h == 3))
                    copy(evq(), yD[:, :, s * 128:(s + 1) * 128], ptb[:, 0:512].rearrange("p (h t) -> p h t", h=4), [pt], [yD],
                         [yD] if s == 0 else [])
                ys = (yA, yB, yC, yD)
                if "YD" in dbg:
                    for i_, nm in enumerate(("YA2", "YB", "YC", "YD")):
                        k.dma("sp", dbgT[nm][:, :, t0:t0 + 512].rearrange("g p t -> p g t"), ys[i_][:], rbuf=ys[i_])
                for fb in range(8):
                    fs = slice(fb * 128, (fb + 1) * 128)
                    ma = r_ma.next()
                    mgo = r_mg.next()
                    for i in range(4):
                        pg = nps()
                        for c in range(8):
                            k.op("pe", lambda e: e.matmul(pg[:], lhsT=wg[:, i, c, fs], rhs=xT[:, c, :], start=(c == 0), stop=(c == 7)),
                                 [wg, xT], [pg], [pg] if c == 0 else [], inc=(c == 7))
                        sg = r_sg.next()
                        k.op("act", lambda e: e.activation(out=sg[:], in_=pg[:], func=AF.Sigmoid), [pg], [sg])
                        pp = nps()
                        for c in range(4):
                            k.op("pe", lambda e: e.matmul(pp[:], lhsT=wbr[:, i, c, fs], rhs=ys[i][:, c, :], start=(c == 0), stop=(c == 3)),
                                 [wbr, ys[i]], [pp], [pp] if c == 0 else [], inc=(c == 3))
                        if i == 0:
                            k.op("dve", lambda e: e.tensor_tensor(out=ma[:], in0=pp[:], in1=sg[:], op=ALU.mult), [pp, sg], [ma])
                        else:
                            mt = r_mt.next()
                            k.op("dve", lambda e: e.tensor_tensor(out=mt[:], in0=pp[:], in1=sg[:], op=ALU.mult), [pp, sg], [mt])
                            if i < 3:
                                k.op("pool", lambda e: e.tensor_tensor(out=ma[:], in0=ma[:], in1=mt[:], op=ALU.add), [ma, mt], [ma], [])
                            else:
                                k.op("pool", lambda e: e.tensor_tensor(out=mgo[:], in0=ma[:], in1=mt[:], op=ALU.add), [ma, mt], [mgo])
                    k.dma("sp", MG[fb, :, t0:t0 + 512], mgo[:], rbuf=mgo)

        def phase2b(l, xsrc):
            wo = k.sbuf("wo", [128, 8, 1024], BF16)
            for c in range(8):
                k.dma("pool", wo[:, c, :], w_out[l, c * 128:(c + 1) * 128, :], wbuf=wo, fresh=(c == 0))
            lg_, lb_ = k.sbuf("ln1g", [128, 1024], F32), k.sbuf("ln1b", [128, 1024], F32)
            k.dma("sp", lg_[:], ln1g[l], wbuf=lg_)
            k.dma("sp", lb_[:], ln1b[l], wbuf=lb_)
            rw = k.sbuf("rw", [128, 8, 32], F32)
            k.dma("sp", rw[:], router_w[l].rearrange("(c p) e -> p c e", p=128), wbuf=rw)
            rwh = k.sbuf("rwh", [128, 8, 32], BF16)
            k.dma("pool", rwh[:], router_w[l].rearrange("(c p) e -> p c e", p=128), wbuf=rwh)
            rwl = k.sbuf("rwl", [128, 8, 32], BF16)
            k.op("dve", lambda e: e.tensor_tensor(out=rwl[:], in0=rw[:], in1=rwh[:], op=ALU.subtract), [rw, rwh], [rwl])
            rb = k.sbuf("rb", [128, 32], F32)
            k.dma("sp", rb[:], router_b[l], wbuf=rb)
            r_mg = Ring(k, "mgl", 2, [128, 8, 512], BF16)
            r_x = Ring(k, "xf", 2, [128, 4, 1024], F32)
            r_rr = Ring(k, "rr", 2, [128, 1024], F32)
            r_st = Ring(k, "st", 4, [128, 32], F32)
            r_x1 = Ring(k, "x1", 2, [128, 1024], F32)
            r_x1b = Ring(k, "x1b", 2, [128, 1024], BF16)
            r_x1l = Ring(k, "x1l", 2, [128, 1024], BF16)
            r_x1Tl = Ring(k, "x1Tl", 2, [128, 8, 128], BF16)
            r_x1Tb = Ring(k, "x1Tb", 2, [128, 8, 512], BF16)
            r_lg = Ring(k, "lgs", 2, [128, 4, 32], F32)
            zt = k.sbuf("zt", [128, 4096], BF16)
            k.op("pool", lambda e: e.memset(zt[:], 0.0), [], [zt])
            for b in range(NSLOT // 512):
                k.dma("act", XS[b * 512:(b + 1) * 512, :].rearrange("(p s) f -> p (s f)", s=4), zt[:], rbuf=zt)
            for i in range(NT):
                t0 = i * 512
                mg = r_mg.next()
                k.dma("sp", mg[:], MG[:, :, t0:t0 + 512].rearrange("c p t -> p c t"), wbuf=mg)
                xf = r_x.next()
                k.dma("act", xf[:], xsrc[t0:t0 + 512, :].rearrange("(s p) f -> p s f", p=128), wbuf=xf)
                x1Tb = r_x1Tb.next()
                lgs = r_lg.next()
                for s in range(4):
                    rr = r_rr.next()
                    for half in range(2):
                        pw_ = nps()
                        for c in range(8):
                            k.op("pe", lambda e: e.matmul(pw_[:], lhsT=mg[:, c, s * 128:(s + 1) * 128], rhs=wo[:, c, half * 512:(half + 1) * 512],
                                                          start=(c == 0), stop=(c == 7)), [mg, wo], [pw_], [pw_] if c == 0 else [], inc=(c == 7))
                        k.op("dve", lambda e: e.scalar_tensor_tensor(out=rr[:, half * 512:(half + 1) * 512], in0=xf[:, s, half * 512:(half + 1) * 512],
                                                                     scalar=ALPHA, in1=pw_[:], op0=ALU.mult, op1=ALU.add),
                             [xf, pw_], [rr], [rr] if half == 0 else [])
                    x1 = r_x1.next()
                    layer_norm_tm(rr, x1, lg_, lb_, r_st)
                    r0 = t0 + s * 128
                    k.dma("sp", X1[r0:r0 + 128, :], x1[:], rbuf=x1)
                    x1b = r_x1b.next()
                    copy("act", x1b[:], x1[:], [x1], [x1b])
                    k.dma("act", X1B[r0:r0 + 128, :], x1b[:], rbuf=x1b)
                    x1l = r_x1l.next()
                    k.op("dve", lambda e: e.tensor_tensor(out=x1l[:], in0=x1[:], in1=x1b[:], op=ALU.subtract), [x1, x1b], [x1l])
                    x1Tl = r_x1Tl.next()
                    for (src, hi) in ((x1b, True), (x1l, False)):
                        pt = nps()
                        ptb = pt[:].bitcast(BF16)
                        for c in range(8):
                            k.op("pe", lambda e: e.transpose(out=ptb[:, c * 128:(c + 1) * 128], in_=src[:, c * 128:(c + 1) * 128], identity=identb[:]),
                                 [src, identb], [pt], [pt] if c == 0 else [], inc=(c == 7))
                        if hi:
                            copy("act", x1Tb[:, :, s * 128:(s + 1) * 128], ptb.rearrange("p (c t) -> p c t", c=8), [pt], [x1Tb],
                                 [x1Tb] if s == 0 else [])
                        else:
                            copy("dve", x1Tl[:], ptb.rearrange("p (c t) -> p c t", c=8), [pt], [x1Tl])
                    pl = nps()
                    n3 = 0
                    for (xa, wa_) in ((0, rwh), (1, rwh), (0, rwl)):
                        for c in range(8):
                            lhs = x1Tb[:, c, s * 128:(s + 1) * 128] if xa == 0 else x1Tl[:, c, :]
                            k.op("pe", lambda e: e.matmul(pl[:, 0:32], lhsT=lhs, rhs=wa_[:, c, :], start=(n3 == 0), stop=(n3 == 23)),
                                 [x1Tb, x1Tl, wa_], [pl], [pl] if n3 == 0 else [], inc=(n3 == 23))
                            n3 += 1
                    k.op("dve", lambda e: e.tensor_tensor(out=lgs[:, s, :], in0=pl[:, 0:32], in1=rb[:], op=ALU.add), [pl, rb], [lgs],
                         [lgs] if s == 0 else [])
                k.dma("sp", X1T[:, :, t0:t0 + 512].rearrange("c p t -> p c t"), x1Tb[:], rbuf=x1Tb)
                k.dma("act", LGD[t0:t0 + 512, :].rearrange("(s p) e -> p s e", p=128), lgs[:], rbuf=lgs)

        def layer_norm_tm(rr, out, g_, b_, r_st, e1="pool", e2="pool"):
            stt = r_st.next()
            for half in range(2):
                k.op("dve", lambda e: e.bn_stats(out=stt[:, half * 6:half * 6 + 6], in_=rr[:, half * 512:(half + 1) * 512]), [rr], [stt],
                     [stt] if half == 0 else [])
            k.op("dve", lambda e: e.bn_aggr(out=stt[:, 12:14], in_=stt[:, 0:12]), [stt], [stt], [])
            k.op("act", lambda e: e.activation(out=stt[:, 14:15], in_=stt[:, 13:14], func=AF.Sqrt, bias=k1_eps[:, 0:1], scale=1.0),
                 [stt, k1_eps], [stt], [])
            k.op("dve", lambda e: e.reciprocal(out=stt[:, 15:16], in_=stt[:, 14:15]), [stt], [stt], [])
            k.op("dve", lambda e: e.tensor_scalar(out=rr[:], in0=rr[:], scalar1=stt[:, 12:13], scalar2=stt[:, 15:16],
                                                  op0=ALU.subtract, op1=ALU.mult), [rr, stt], [rr], [])
            k.op(e1, lambda e: e.tensor_tensor(out=rr[:], in0=rr[:], in1=g_[:], op=ALU.mult), [rr, g_], [rr], [])
            k.op(e2, lambda e: e.tensor_tensor(out=out[:], in0=rr[:], in1=b_[:], op=ALU.add), [rr, b_], [out])


        NTT = T // 128
        LOGBLK = BLK.bit_length() - 1
        NSUB = BLK // 128

        def phase3(l):
            DKi = k.sbuf("DKi", [128, NTT, 4], I32)
            GUI = k.sbuf("GUI", [128, NB, 8], I32)
            BGI = k.sbuf("BGI", [128, NB], I32)
            BDI = k.sbuf("BDI", [128, NB], I32)
            with k.scope():
                route(l, DKi, GUI, BGI, BDI)
            with k.scope():
                dispatch(DKi)
            with k.scope():
                experts(l, GUI, BGI, BDI)

        def route(l, DKi, GUI, BGI, BDI):
            LG = k.sbuf("LG", [128, NTT, 32], F32)
            k.dma("sp", LG[:], LGD.rearrange("(n p) e -> p n e", p=128), wbuf=LG)
            mx8 = k.sbuf("mx8", [128, NTT, 8], F32)
            for n in range(NTT):
                k.op("dve", lambda e: e.max(out=mx8[:, n, :], in_=LG[:, n, :]), [LG], [mx8], [mx8] if n == 0 else [])
            M = k.sbuf("Mk", [128, NTT, 32], BF16)
            k.op("dve", lambda e: e.tensor_tensor(out=M[:], in0=LG[:], in1=mx8[:, :, 3:4].to_broadcast([128, NTT, 32]), op=ALU.is_ge),
                 [LG, mx8], [M])
            trilb = k.sbuf("trilb", [128, 128], BF16)
            k.dma("pool", trilb[:], tril, wbuf=trilb)
            onesb = k.sbuf("onesb", [128, 128], BF16)
            k.op("dve", lambda e: e.memset(onesb[:], 1.0), [], [onesb])
            RK = k.sbuf("RK", [128, NTT, 32], F32)
            CT = k.sbuf("CT", [128, NTT, 32], F32)
            n0 = 0
            while n0 < NTT:
                w = min(16, NTT - n0)
                for (lhs, dst) in ((trilb, RK), (onesb, CT)):
                    pm = nps()
                    k.op("pe", lambda e: e.matmul(pm[:, 0:w * 32], lhsT=lhs[:], rhs=M[:, n0:n0 + w, :].rearrange("p n e -> p (n e)"),
                                                  start=True, stop=True), [lhs, M], [pm])
                    copy(evq(), dst[:, n0:n0 + w, :].rearrange("p n e -> p (n e)"), pm[:, 0:w * 32], [pm], [dst], [dst] if n0 == 0 else [])
                n0 += w
            PRE = k.sbuf("PRE", [128, NTT + 1, 32], F32)
            k.op("dve", lambda e: e.memset(PRE[:, 0, :], 0.0), [], [PRE])
            for n in range(NTT):
                k.op("dve", lambda e: e.tensor_tensor(out=PRE[:, n + 1, :], in0=PRE[:, n, :], in1=CT[:, n, :], op=ALU.add), [PRE, CT], [PRE], [])
            sm = k.sbuf("rsm", [128, 8, 32], F32)
            smi = k.sbuf("rsmi", [128, 2, 32], I32)
            TOT = PRE[:, NTT, :]
            k.op("dve", lambda e: e.tensor_scalar(out=smi[:, 0, :], in0=TOT, scalar1=float(BLK - 1), scalar2=None, op0=ALU.add), [PRE], [smi])
            k.op("dve", lambda e: e.tensor_scalar(out=smi[:, 1, :], in0=smi[:, 0, :], scalar1=LOGBLK, scalar2=None, op0=ALU.arith_shift_right),
                 [smi], [smi], [])
            k.op("dve", lambda e: e.tensor_scalar(out=smi[:, 0, :], in0=smi[:, 1, :], scalar1=LOGBLK, scalar2=None, op0=ALU.logical_shift_left),
                 [smi], [smi], [])
            PAD, ENDS, START, ONES = sm[:, 0, :], sm[:, 1, :], sm[:, 2, :], sm[:, 3, :]
            k.op("dve", lambda e: e.tensor_copy(out=PAD, in_=smi[:, 0, :]), [smi], [sm])
            k.op("dve", lambda e: e.memset(ONES, 1.0), [], [sm], [])
            k.op("dve", lambda e: e.tensor_tensor_scan(out=ENDS, data0=ONES, data1=PAD, initial=0.0, op0=ALU.mult, op1=ALU.add), [sm], [sm], [])
            k.op("dve", lambda e: e.tensor_tensor(out=START, in0=ENDS, in1=PAD, op=ALU.subtract), [sm], [sm], [])
            k.op("dve", lambda e: e.tensor_tensor(out=RK[:], in0=RK[:], in1=PRE[:, 0:NTT, :], op=ALU.add), [RK, PRE], [RK], [])
            k.op("dve", lambda e: e.tensor_tensor(out=RK[:], in0=RK[:], in1=sm[:, 2:3, :].to_broadcast([128, NTT, 32]), op=ALU.add),
                 [RK, sm], [RK], [])
            EQ = k.sbuf("EQ", [128, NTT, 32], F32)
            DK = k.sbuf("DK", [128, NTT, 4], F32)
            for kk in range(4):
                k.op("dve", lambda e: e.tensor_tensor(out=EQ[:], in0=LG[:], in1=mx8[:, :, kk:kk + 1].to_broadcast([128, NTT, 32]),
                                                      op=ALU.is_equal), [LG, mx8], [EQ])
                k.op("dve", lambda e: e.tensor_tensor(out=EQ[:], in0=EQ[:], in1=RK[:], op=ALU.mult), [EQ, RK], [EQ], [])
                k.op("dve", lambda e: e.tensor_reduce(out=DK[:, :, kk], in_=EQ[:], axis=AX.X, op=ALU.add), [EQ], [DK], [DK] if kk == 0 else [])
            k.op("dve", lambda e: e.tensor_copy(out=DKi[:], in_=DK[:]), [DK], [DKi])
            k.dma("sp", DESTD.rearrange("(n p) c -> p n c", p=128), DKi[:], rbuf=DKi)
            PR = k.sbuf("PR", [128, NTT, 4], F32)
            PS_ = k.sbuf("PS_", [128, 2, NTT], F32)
            k.op("dve", lambda e: e.tensor_tensor(out=PR[:], in0=mx8[:, :, 0:4], in1=mx8[:, :, 0:1].to_broadcast([128, NTT, 4]), op=ALU.subtract),
                 [mx8], [PR])
            k.op("act", lambda e: e.activation(out=PR[:], in_=PR[:], func=AF.Exp), [PR], [PR], [])
            k.op("dve", lambda e: e.tensor_reduce(out=PS_[:, 0, :], in_=PR[:], axis=AX.X, op=ALU.add), [PR], [PS_])
            k.op("dve", lambda e: e.reciprocal(out=PS_[:, 1, :], in_=PS_[:, 0, :]), [PS_], [PS_], [])
            k.op("dve", lambda e: e.tensor_tensor(out=PR[:], in0=PR[:], in1=PS_[:, 1, :].unsqueeze(2).to_broadcast([128, NTT, 4]), op=ALU.mult),
                 [PR, PS_], [PR], [])
            k.dma("sp", PROBD.rearrange("(n p) c -> p n c", p=128), PR[:], rbuf=PR)
            IOi = k.sbuf("IOi", [128, NB + 16], I32)
            k.op("pool", lambda e: e.iota(IOi[:, 0:NB], pattern=[[BLK, NB]], base=0, channel_multiplier=0), [], [IOi])
            k.op("pool", lambda e: e.iota(IOi[:, NB:NB + 8], pattern=[[128, 8]], base=0, channel_multiplier=1), [], [IOi], [])
            IOf = k.sbuf("IOf", [128, NB + 16], F32)
            k.op("dve", lambda e: e.tensor_copy(out=IOf[:, 0:NB + 8], in_=IOi[:, 0:NB + 8]), [IOi], [IOf])
            BE = k.sbuf("BE", [128, 3, NB], F32)
            k.op("dve", lambda e: e.tensor_scalar(out=BE[:, 0, :], in0=IOf[:, 0:NB], scalar1=sm[:, 1, 0:1], scalar2=None, op0=ALU.is_ge), [IOf, sm], [BE])
            for ee in range(1, 32):
                k.op("dve", lambda e: e.scalar_tensor_tensor(out=BE[:, 0, :], in0=IOf[:, 0:NB], scalar=sm[:, 1, ee:ee + 1], in1=BE[:, 0, :],
                                                             op0=ALU.is_ge, op1=ALU.add), [IOf, sm, BE], [BE], [])
            k.op("dve", lambda e: e.tensor_scalar(out=BE[:, 0, :], in0=BE[:, 0, :], scalar1=31.0, scalar2=float(l * E), op0=ALU.min, op1=ALU.add),
                 [BE], [BE], [])
            k.op("dve", lambda e: e.tensor_copy(out=BDI[:], in_=BE[:, 0, :]), [BE], [BDI])
            k.op("dve", lambda e: e.tensor_scalar(out=BE[:, 1, :], in0=BE[:, 0, :], scalar1=128.0, scalar2=IOf[:, NB:NB + 1], op0=ALU.mult, op1=ALU.add),
                 [BE, IOf], [BE], [])
            k.op("dve", lambda e: e.tensor_copy(out=BGI[:], in_=BE[:, 1, :]), [BE], [BGI])
            GUf = k.sbuf("GUf", [128, NB, 8], F32)
            for c in range(8):
                k.op("dve", lambda e: e.tensor_scalar(out=GUf[:, :, c], in0=BE[:, 0, :], scalar1=1024.0, scalar2=IOf[:, NB + c:NB + c + 1],
                                                      op0=ALU.mult, op1=ALU.add), [BE, IOf], [GUf], [GUf] if c == 0 else [])
            k.op("dve", lambda e: e.tensor_copy(out=GUI[:], in_=GUf[:]), [GUf], [GUI])
            if "BED" in dbg:
                k.dma("sp", dbgT["BED"], BDI[0:1, :], rbuf=BDI)

        def dispatch(DKi):
            r_xb = Ring(k, "dxb", 3, [128, 1024], BF16)
            for n in range(NTT):
                xb = r_xb.next()
                k.dma("pool", xb[:], X1B[n * 128:(n + 1) * 128, :], wbuf=xb)
                for kk in range(4):
                    k.dma("pool", None, None, rbuf=xb, extra_reads=[DKi],
                          fn=lambda e: e.indirect_dma_start(out=XS, out_offset=bass.IndirectOffsetOnAxis(ap=DKi[:, n, kk:kk + 1], axis=0),
                                                            in_=xb[:, :], in_offset=None))

        def experts(l, GUI, BGI, BDI):
            r_wgu = Ring(k, "wgu", 2, [128, 8, 2048], BF16)
            r_wdn = Ring(k, "wdn", 2, [128, 8, 1024], BF16)
            r_bgu = Ring(k, "bgu", 2, [128, 32], F32)
            r_bdn = Ring(k, "bdn", 2, [128, 1024], F32)
            r_xs = Ring(k, "exs", 2, [128, NSUB, 1024], BF16)
            r_xsT = Ring(k, "exsT", 1, [128, 8, BLK], BF16)
            r_act = Ring(k, "eact", 2, [128, 8, BLK], BF16)
            r_g = Ring(k, "eg", 2, [128, BLK], F32)
            r_s = Ring(k, "es", 2, [128, BLK], F32)
            r_u = Ring(k, "eu", 2, [128, BLK], F32)
            r_y = Ring(k, "ey", 2, [128, 1024], F32)
            def load_block(b):
                wgu, wdn, bgu, bdn = r_wgu.next(), r_wdn.next(), r_bgu.next(), r_bdn.next()
                for c in range(8):
                    k.dma("pool", None, None, wbuf=wgu, fresh=(c == 0), extra_reads=[GUI],
                          fn=lambda e: e.indirect_dma_start(out=wgu[:, c, :], out_offset=None, in_=w_gu,
                                                            in_offset=bass.IndirectOffsetOnAxis(ap=GUI[:, b, c:c + 1], axis=0)))
                    k.dma("pool", None, None, wbuf=wdn, fresh=(c == 0), extra_reads=[GUI],
                          fn=lambda e: e.indirect_dma_start(out=wdn[:, c, :], out_offset=None, in_=w_dn,
                                                            in_offset=bass.IndirectOffsetOnAxis(ap=GUI[:, b, c:c + 1], axis=0)))
                k.dma("pool", None, None, wbuf=bgu, extra_reads=[BGI],
                      fn=lambda e: e.indirect_dma_start(out=bgu[:, 0:16], out_offset=None, in_=b_gu,
                                                        in_offset=bass.IndirectOffsetOnAxis(ap=BGI[:, b:b + 1], axis=0)))
                k.dma("pool", None, None, wbuf=bdn, extra_reads=[BDI],
                      fn=lambda e: e.indirect_dma_start(out=bdn[:, :], out_offset=None, in_=b_dn,
                                                        in_offset=bass.IndirectOffsetOnAxis(ap=BDI[:, b:b + 1], axis=0)))
                xs = r_xs.next()
                k.dma("sp", xs[:], XS[b * BLK:(b + 1) * BLK, :].rearrange("(s p) f -> p s f", p=128), wbuf=xs)
                return wgu, wdn, bgu, bdn, xs

            def compute_block(b, bufs):
                wgu, wdn, bgu, bdn, xs = bufs
                bg1 = r_bg1.next()
                k.op("dve", lambda e: e.tensor_scalar(out=bg1[:, 0:8], in0=bgu[:, 8:16], scalar1=1.0, scalar2=None, op0=ALU.add), [bgu], [bg1])
                xsT = r_xsT.next()
                for s in range(NSUB):
                    pt = nps()
                    ptb = pt[:].bitcast(BF16)
                    for c in range(8):
                        k.op("pe", lambda e: e.transpose(out=ptb[:, c * 128:(c + 1) * 128], in_=xs[:, s, c * 128:(c + 1) * 128], identity=identb[:]),
                             [xs, identb], [pt], [pt] if c == 0 else [], inc=(c == 7))
                    copy(evq(), xsT[:, :, s * 128:(s + 1) * 128], ptb.rearrange("p (c t) -> p c t", c=8), [pt], [xsT], [xsT] if s == 0 else [])
                act = r_act.next()
                for j in range(8):
                    pgs = []
                    for jj in (j, 8 + j):
                        pm = nps()
                        for c in range(8):
                            k.op("pe", lambda e: e.matmul(pm[:, 0:BLK], lhsT=wgu[:, c, jj * 128:(jj + 1) * 128], rhs=xsT[:, c, :],
                                                          start=(c == 0), stop=(c == 7)), [wgu, xsT], [pm], [pm] if c == 0 else [], inc=(c == 7))
                        pgs.append(pm)
                    gt, st_, ut = r_g.next(), r_s.next(), r_u.next()
                    k.op("dve", lambda e: e.tensor_scalar(out=gt[:], in0=pgs[0][:, 0:BLK], scalar1=bgu[:, j:j + 1], scalar2=7.0, op0=ALU.add, op1=ALU.min),
                         [pgs[0], bgu], [gt])
                    k.op("act", lambda e: e.activation(out=st_[:], in_=gt[:], func=AF.Sigmoid, scale=1.702), [gt], [st_])
                    k.op("dve", lambda e: e.tensor_scalar(out=ut[:], in0=pgs[1][:, 0:BLK], scalar1=bg1[:, j:j + 1], scalar2=8.0, op0=ALU.add, op1=ALU.min),
                         [pgs[1], bg1], [ut])
                    k.op("dve", lambda e: e.scalar_tensor_tensor(out=ut[:], in0=ut[:], scalar=-6.0, in1=gt[:], op0=ALU.max, op1=ALU.mult), [ut, gt], [ut], [])
                    k.op("dve", lambda e: e.tensor_tensor(out=act[:, j, :], in0=ut[:], in1=st_[:], op=ALU.mult), [ut, st_], [act], [act] if j == 0 else [])
                for s in range(NSUB):
                    yo = r_y.next()
                    for half in range(2):
                        pd = nps()
                        for c in range(8):
                            k.op("pe", lambda e: e.matmul(pd[:], lhsT=act[:, c, s * 128:(s + 1) * 128], rhs=wdn[:, c, half * 512:(half + 1) * 512],
                                                          start=(c == 0), stop=(c == 7)), [act, wdn], [pd], [pd] if c == 0 else [], inc=(c == 7))
                        k.op("dve", lambda e: e.tensor_tensor(out=yo[:, half * 512:(half + 1) * 512], in0=pd[:], in1=bdn[:, half * 512:(half + 1) * 512],
                                                              op=ALU.add), [pd, bdn], [yo], [yo] if half == 0 else [])
                    r0 = b * BLK + s * 128
                    for half in range(2):
                        k.dma("sp", YSH[half][r0:r0 + 128, :], yo[:, half * 512:(half + 1) * 512], rbuf=yo)

            r_bg1 = Ring(k, "bg1", 2, [128, 8], F32)
            pend = load_block(0)
            for b in range(NB):
                nxt = load_block(b + 1) if b + 1 < NB else None
                compute_block(b, pend)
                pend = nxt

        def phase4(l, dst):
            wpg = k.sbuf("wpg", [128, 8, 1024], BF16)
            for c in range(8):
                k.dma("pool", wpg[:, c, :], w_pg[l, c * 128:(c + 1) * 128, :], wbuf=wpg, fresh=(c == 0))
            wpp = k.sbuf("wpp", [128, 2, 1024], BF16)
            k.dma("pool", wpp[:], w_pp[l].rearrange("(c p) n -> p c n", p=128), wbuf=wpp)
            lg_, lb_ = k.sbuf("ln2g", [128, 1024], F32), k.sbuf("ln2b", [128, 1024], F32)
            k.dma("sp", lg_[:], ln2g[l], wbuf=lg_)
            k.dma("sp", lb_[:], ln2b[l], wbuf=lb_)
            DKi = k.sbuf("DKi4", [128, NTT, 4], I32)
            k.dma("sp", DKi[:], DESTD.rearrange("(n p) c -> p n c", p=128), wbuf=DKi)
            PR = k.sbuf("PR4", [128, NTT, 4], F32)
            k.dma("sp", PR[:], PROBD.rearrange("(n p) c -> p n c", p=128), wbuf=PR)
            r_x1 = Ring(k, "fx1", 2, [128, 1024], F32)
            r_xT = Ring(k, "fxT", 2, [128, 8, 128], BF16)
            r_pb = Ring(k, "fpb", 2, [128, 256], BF16)
            r_pf = Ring(k, "fpf", 2, [128, 256], F32)
            r_pT = Ring(k, "fpT", 2, [128, 2, 128], BF16)
            r_yk = Ring(k, "fyk", 2, [128, 4, 1024], F32)
            r_sg = Ring(k, "fsg", 2, [128, 1024], F32)
            r_acc = Ring(k, "facc", 2, [128, 1024], F32)
            r_o = Ring(k, "fo", 2, [128, 1024], F32)
            r_st = Ring(k, "fst", 4, [128, 32], F32)
            def load4(n):
                r0 = n * 128
                x1 = r_x1.next()
                k.dma("sp", x1[:], X1[r0:r0 + 128, :], wbuf=x1)
                xT = r_xT.next()
                k.dma("act", xT[:], X1T[:, :, r0:r0 + 128].rearrange("c p t -> p c t"), wbuf=xT)
                pf = r_pf.next()
                k.dma("act", pf[:], p_in[l, r0:r0 + 128, :], wbuf=pf)
                pb = pf
                yk = r_yk.next()
                for kk in range(4):
                    for half in range(2):
                        k.dma("pool", None, None, wbuf=yk, fresh=(kk == 0 and half == 0), extra_reads=[DKi],
                              fn=lambda e: e.indirect_dma_start(out=yk[:, kk, half * 512:(half + 1) * 512], out_offset=None, in_=YSH[half],
                                                                in_offset=bass.IndirectOffsetOnAxis(ap=DKi[:, n, kk:kk + 1], axis=0)))
                return x1, xT, pb, yk

            pend4 = load4(0)
            for n in range(NTT):
                r0 = n * 128
                x1, xT, pb, yk = pend4
                if n + 1 < NTT:
                    pend4 = load4(n + 1)
                pf = pb
                pb = r_pb.next()
                copy("act", pb[:], pf[:], [pf], [pb])
                pt = nps()
                ptb = pt[:].bitcast(BF16)
                for c in range(2):
                    k.op("pe", lambda e: e.transpose(out=ptb[:, c * 128:(c + 1) * 128], in_=pb[:, c * 128:(c + 1) * 128], identity=identb[:]),
                         [pb, identb], [pt], [pt] if c == 0 else [], inc=(c == 1))
                pT = r_pT.next()
                copy("act", pT[:], ptb[:, 0:256].rearrange("p (c t) -> p c t", c=2), [pt], [pT])
                sg, acc = r_sg.next(), r_acc.next()
                for half in range(2):
                    hs = slice(half * 512, (half + 1) * 512)
                    pg = nps()
                    for c in range(8):
                        k.op("pe", lambda e: e.matmul(pg[:], lhsT=xT[:, c, :], rhs=wpg[:, c, hs], start=(c == 0), stop=(c == 7)),
                             [xT, wpg], [pg], [pg] if c == 0 else [], inc=(c == 7))
                    k.op("act", lambda e: e.activation(out=sg[:, hs], in_=pg[:], func=AF.Sigmoid), [pg], [sg], [sg] if half == 0 else [])
                    pp = nps()
                    for c in range(2):
                        k.op("pe", lambda e: e.matmul(pp[:], lhsT=pT[:, c, :], rhs=wpp[:, c, hs], start=(c == 0), stop=(c == 1)),
                             [pT, wpp], [pp], [pp] if c == 0 else [], inc=(c == 1))
                    k.op("dve", lambda e: e.tensor_tensor(out=acc[:, hs], in0=pp[:], in1=sg[:, hs], op=ALU.mult), [pp, sg], [acc], [acc] if half == 0 else [])
                k.op("dve", lambda e: e.scalar_tensor_tensor(out=acc[:], in0=x1[:], scalar=ALPHA, in1=acc[:], op0=ALU.mult, op1=ALU.add), [x1, acc], [acc], [])
                for kk in range(4):
                    k.op("dve", lambda e: e.scalar_tensor_tensor(out=acc[:], in0=yk[:, kk, :], scalar=PR[:, n, kk:kk + 1], in1=acc[:],
                                                                 op0=ALU.mult, op1=ALU.add), [yk, PR, acc], [acc], [])
                o = r_o.next()
                layer_norm_tm(acc, o, lg_, lb_, r_st, e1="dve", e2="pool")
                k.dma("sp", dst[r0:r0 + 128, :], o[:], rbuf=o)


        k1_eps = k.sbuf("k1_eps", [128, 1], F32)
        k.op("dve", lambda g: g.memset(k1_eps[:], LN_EPS), [], [k1_eps])

        for l in range(NL):
            xsrc = x_in if l == 0 else XN
            if "p1" in phases:
                with k.scope():
                    phase1(l, xsrc)
            if "pa" in phases:
                with k.scope():
                    phaseA(l)
            if "p2a" in phases:
                with k.scope():
                    phase2a(l, xsrc)
            if "p2b" in phases:
                with k.scope():
                    phase2b(l, xsrc)
            if "p3" in phases:
                with k.scope():
                    phase3(l)
            if "p4" in phases:
                with k.scope():
                    phase4(l, y_out if l == NL - 1 else XN)
        k.barrier()
    return nc


_last_raw = None


def random_weights(rng, NL=2):
    f = np.float32

    def nrm(shape, s):
        return (rng.standard_normal(shape) * s).astype(f)
    beta = 16.0 ** -0.25
    return {
        'w_in': nrm((NL, D, NIN), D ** -0.5), 'spatial_w': nrm((NL, 4, 128, 128), 128 ** -0.5),
        'spatial_b': 1.0 + nrm((NL, 4, 128), 0.02), 'conv_w': nrm((NL, 3, 512), 3 ** -0.5),
        'pool_w': nrm((NL, 4, 128, 128), 128 ** -0.5), 'pool_scale': 1.0 + nrm((NL, 512), 0.02),
        'w_branch': nrm((NL, 4, 512, D), 512 ** -0.5), 'w_gate': nrm((NL, 4, D, D), D ** -0.5),
        'w_out': nrm((NL, D, D), beta * D ** -0.5), 'ln1_g': 1.0 + nrm((NL, D), 0.02), 'ln1_b': nrm((NL, D), 0.02),
        'router_w': nrm((NL, D, E), D ** -0.5), 'router_b': nrm((NL, E), 0.01),
        'w_gate_up': nrm((NL, E, D, 2 * FF), D ** -0.5), 'b_gate_up': nrm((NL, E, 2 * FF), 0.02),
        'w_down': nrm((NL, E, FF, D), beta * FF ** -0.5), 'b_down': nrm((NL, E, D), 0.02),
        'w_ple_gate': nrm((NL, D, D), D ** -0.5), 'w_ple_proj': nrm((NL, PLE, D), beta * PLE ** -0.5),
        'ln2_g': 1.0 + nrm((NL, D), 0.02), 'ln2_b': nrm((NL, D), 0.02),
    }


def make_weights_inputs(W, NL=2):
    global _last_raw
    _last_raw = W
    f = np.float32
    c = np.ascontiguousarray

    def rep(a):
        a = np.asarray(a, f)
        return c(np.broadcast_to(a[:, None, :], (a.shape[0], 128, a.shape[1])))
    out = {
        "w_in": c(np.asarray(W['w_in'], f)),
        "spT": c(np.asarray(W['spatial_w'], f).transpose(0, 3, 1, 2)),
        "sbias": c(np.broadcast_to(np.asarray(W['spatial_b'], f)[:, None], (NL, 128, 4, 128))),
        "convw": c(np.asarray(W['conv_w'], f).reshape(NL, 3, 4, 128).transpose(0, 3, 1, 2)),
        "poolw": c(np.asarray(W['pool_w'], f).transpose(0, 2, 1, 3)),
        "pscale": c(np.asarray(W['pool_scale'], f).reshape(NL, 4, 128).transpose(0, 2, 1)),
        "w_branch": c(np.asarray(W['w_branch'], f)), "w_gate": c(np.asarray(W['w_gate'], f)),
        "w_out": c(np.asarray(W['w_out'], f)),
        "ln1g": rep(W['ln1_g']), "ln1b": rep(W['ln1_b']),
        "router_w": c(np.asarray(W['router_w'], f)), "router_b": rep(W['router_b']),
        "w_gu": np.asarray(W['w_gate_up'], f).reshape(NL * E * D, 2 * FF),
        "b_gu": c(np.asarray(W['b_gate_up'], f).reshape(NL, E, 16, 128).transpose(0, 1, 3, 2)).reshape(NL * E * 128, 16),
        "w_dn": np.asarray(W['w_down'], f).reshape(NL * E * FF, D),
        "b_dn": np.asarray(W['b_down'], f).reshape(NL * E, D),
        "w_pg": c(np.asarray(W['w_ple_gate'], f)), "w_pp": c(np.asarray(W['w_ple_proj'], f)),
        "ln2g": rep(W['ln2_g']), "ln2b": rep(W['ln2_b']),
        "identf": np.eye(128, dtype=f),
        "abias": att_bias_tables().reshape(128, -1),
        "pcorr": pool_corr().reshape(128, -1),
        "tril": np.triu(np.ones((128, 128), f), 1),
    }
    return out


def kernel(x_prompt, x_sample, p_prompt, p_sample, w_in, spatial_w, spatial_b, conv_w, pool_w, pool_scale,
           w_branch, w_gate, w_out, ln1_g, ln1_b, router_w, router_b, w_gate_up, b_gate_up, w_down, b_down,
           w_ple_gate, w_ple_proj, ln2_g, ln2_b):
    NCORE = 8
    f = np.float32
    x_prompt, x_sample = np.asarray(x_prompt, f), np.asarray(x_sample, f)
    p_prompt, p_sample = np.asarray(p_prompt, f), np.asarray(p_sample, f)
    NL = p_prompt.shape[0]
    B, S, _ = x_prompt.shape
    DB, DS, _ = x_sample.shape
    per = DB // NCORE
    seqs = [S] + [DS] * per
    W = dict(w_in=w_in, spatial_w=spatial_w, spatial_b=spatial_b, conv_w=conv_w, pool_w=pool_w, pool_scale=pool_scale,
             w_branch=w_branch, w_gate=w_gate, w_out=w_out, ln1_g=ln1_g, ln1_b=ln1_b, router_w=router_w, router_b=router_b,
             w_gate_up=w_gate_up, b_gate_up=b_gate_up, w_down=w_down, b_down=b_down, w_ple_gate=w_ple_gate,
             w_ple_proj=w_ple_proj, ln2_g=ln2_g, ln2_b=ln2_b)
    shared = make_weights_inputs(W, NL=NL)
    in_maps = []
    for c in range(NCORE):
        m = dict(shared)
        m["x"] = np.ascontiguousarray(np.concatenate([x_prompt[c], x_sample[c * per:(c + 1) * per].reshape(per * DS, D)], 0))
        m["p"] = np.ascontiguousarray(np.concatenate([p_prompt[:, c], p_sample[:, c * per:(c + 1) * per].reshape(NL, per * DS, PLE)], 1))
        in_maps.append(m)
    nc = build(seqs, NL=NL)
    res = run_bass_kernel_spmd(nc, in_maps, core_ids=list(range(NCORE)))
    y_prompt = np.empty((B, S, D), f)
    y_sample = np.empty((DB, DS, D), f)
    for c in range(NCORE):
        y = np.asarray(res.results[c]["y"])
        y_prompt[c] = y[:S]
        y_sample[c * per:(c + 1) * per] = y[S:].reshape(per, DS, D)
    return (y_prompt, y_sample)
```

```python
import contextlib
import numpy as np
import concourse.bass as bass
import concourse.mybir as mybir
from concourse.bass_utils import run_bass_kernel_spmd

F32 = mybir.dt.float32
BF16 = mybir.dt.bfloat16
I32 = mybir.dt.int32
AF = mybir.ActivationFunctionType
ALU = mybir.AluOpType
AX = mybir.AxisListType

D = 1024
NIN = 7680
E = 32
FF = 1024
PLE = 256
ALPHA = 4.0 ** 0.25
LN_EPS = 1e-5
ATT = ((128, 1), (512, 4), (2048, 16))
POOLW = (2, 4, 8, 16)
NEG = -1.0e30
SCALE = 128.0 ** -0.5


class Buf:
    def __init__(self, t, name):
        self.t = t
        self.name = name
        self.writers = {}
        self.readers = {}
        self.war = {}
        self.dsem = None
        self.dcnt = 0

    def __getitem__(self, key):
        return self.t[key]


def _merge(dst, src):
    for s, v in src.items():
        if dst.get(s, 0) < v:
            dst[s] = v


class K:
    def __init__(self, nc, stack):
        self.nc = nc
        self.stack = stack
        self.eng = {"pe": nc.tensor, "act": nc.scalar, "dve": nc.vector, "pool": nc.gpsimd, "sp": nc.sync}
        self.esem, self.ecnt, self.seen, self.sems, self.ekey = {}, {}, {}, {}, {}
        self.uid = 0
        for e in self.eng:
            s = stack.enter_context(nc.semaphore("es_" + e))
            self.esem[e] = s
            self.ekey[e] = self._key(s)
            self.ecnt[e] = 0
            self.seen[e] = {}
        self.bufs = []
        self.dpool = {True: [], False: []}
        self.root = stack

    def _key(self, s):
        self.uid += 1
        self.sems[self.uid] = s
        return self.uid

    @contextlib.contextmanager
    def scope(self):
        old = self.stack
        nb = len(self.bufs)
        with contextlib.ExitStack() as sub:
            self.stack = sub
            yield
            self.barrier()
            for b in self.bufs[nb:]:
                if b.dsem is not None:
                    self.dpool[b.dsw].append((b.dsem, b.dkey, b.dcnt))
            del self.bufs[nb:]
        self.stack = old

    def sbuf(self, name, shape, dt):
        self.uid += 1
        t = self.stack.enter_context(self.nc.sbuf_tensor(f"{name}_{self.uid}", list(shape), dt))
        b = Buf(t, name)
        self.bufs.append(b)
        return b

    def psum(self, name, shape, dt):
        t = self.stack.enter_context(self.nc.psum_tensor(name, list(shape), dt))
        b = Buf(t, name)
        self.bufs.append(b)
        return b

    def _dsem(self, b, sw):
        if b.dsem is None:
            pool = self.dpool[sw]
            if pool:
                b.dsem, b.dkey, b.dcnt = pool.pop()
                b.dsw = sw
            else:
                b.dsw = sw
                self.uid += 1
                s = self.root.enter_context(self.nc.semaphore(f"ds{self.uid}"))
                b.dsem = s
                b.dkey = self._key(s)
        assert b.dsw == sw, (b.name, 'mixed SW/HW DMA queues on one buffer')
        return b.dsem

    def _wait(self, e, conds):
        seen = self.seen[e]
        for sid, v in conds.items():
            if seen.get(sid, 0) < v:
                self.eng[e].wait_ge(self.sems[sid], v)
                seen[sid] = v

    def _deps(self, e, reads, writes, fresh):
        conds = {}
        for b in reads:
            _merge(conds, b.writers)
        for b in writes:
            if b in fresh:
                w = {}
                _merge(w, b.readers)
                _merge(w, b.writers)
                b.war = w
                b.writers = {}
                b.readers = {}
            _merge(conds, b.war)
        self._wait(e, conds)

    def op(self, e, fn, reads=(), writes=(), fresh=None, inc=True):
        if fresh is None:
            fresh = writes
        self._deps(e, reads, writes, fresh)
        ins = fn(self.eng[e])
        if inc:
            self.ecnt[e] += 1
            ins.then_inc(self.esem[e], 1)
            val = self.ecnt[e]
        else:
            val = self.ecnt[e] + 1
        c = {self.ekey[e]: val}
        for b in reads:
            _merge(b.readers, c)
        for b in writes:
            _merge(b.writers, c)
        return ins

    def dma(self, q, out, in_, wbuf=None, rbuf=None, fresh=True, extra_reads=(), fn=None):
        reads = list(extra_reads) + ([rbuf] if rbuf is not None else [])
        writes = [wbuf] if wbuf is not None else []
        self._deps(q, reads, writes, writes if fresh else [])
        tb = wbuf if wbuf is not None else (rbuf if rbuf is not None else extra_reads[0])
        s = self._dsem(tb, q == 'pool')
        tb.dcnt += 16
        if fn is None:
            ins = self.eng[q].dma_start(out=out, in_=in_)
        else:
            ins = fn(self.eng[q])
        ins.then_inc(s, 16)
        c = {tb.dkey: tb.dcnt}
        for b in reads:
            _merge(b.readers, c)
        for b in writes:
            _merge(b.writers, c)
        return ins

    def barrier(self):
        conds = {}
        for b in self.bufs:
            _merge(conds, b.writers)
            _merge(conds, b.readers)
            _merge(conds, b.war)
        for e in self.eng:
            if self.ecnt[e] > 0:
                _merge(conds, {self.ekey[e]: self.ecnt[e]})
        for e in self.eng:
            self._wait(e, conds)


class Ring:
    def __init__(self, k, name, n, shape, dt, psum=False):
        mk = k.psum if psum else k.sbuf
        self.b = [mk(f"{name}{i}", shape, dt) for i in range(n)]
        self.i = 0

    def next(self):
        b = self.b[self.i % len(self.b)]
        self.i += 1
        return b


def alibi_slopes():
    n = 12
    return (2.0 ** (-8.0 * np.arange(1, n + 1) / n)).reshape(3, 4)


def att_bias_tables():
    q = np.arange(128)[:, None]
    kk = np.arange(256)[None, :] - 64
    rel = kk - q
    band = np.abs(rel) <= 64
    sl = alibi_slopes()
    out = np.zeros((128, 4, 3, 4, 256), np.float32)
    for var in range(4):
        valid = band.copy()
        if var & 1:
            valid &= (kk >= 0)
        if var & 2:
            valid &= (kk < 128)
        for g, (_, dil) in enumerate(ATT):
            for h in range(4):
                b = -sl[g, h] * np.abs(rel) * dil
                out[:, var, g, h, :] = np.where(valid, b, NEG)
    return out


def pool_corr():
    out = np.ones((2, 4, 8), np.float32)
    Sx = 4096
    for g, w in enumerate(POOLW):
        t = np.arange(8)
        out[0, g] = w / (t + w // 2 - np.maximum(t - w // 2, 0))
        t = Sx - 8 + np.arange(8)
        out[1, g] = w / (np.minimum(t + w // 2, Sx) - (t - w // 2))
    return np.broadcast_to(out[None], (128, 2, 4, 8)).copy()


def pool_invcount(S):
    out = np.zeros((3, 4, 512), np.float32)
    for g, w in enumerate(POOLW):
        out[0, g, :] = 1.0 / w
        t = np.arange(512)
        out[1, g, :] = 1.0 / (np.minimum(t + w // 2, 10 ** 9) - np.maximum(t - w // 2, 0))
        tt = np.arange(512) + 1024
        out[2, g, :] = 1.0 / (np.minimum(tt + w // 2, 1536) - (tt - w // 2))
    return np.broadcast_to(out[None], (128, 3, 4, 512)).copy()


def build(seqs, NL=2, BLK=512, dbg=(), phases=("p1", "pa", "p2a", "p2b", "p3", "p4")):
    T = sum(seqs)
    NT = T // 512
    A = T * 4
    NB = -(-(A + E * (BLK - 1)) // BLK)
    NSLOT = NB * BLK
    nc = bass.Bass("TRN2", target_bir_lowering=False)

    def din(name, shape, dt=F32):
        return nc.dram_tensor(name, list(shape), dt, kind="ExternalInput").ap()

    def dscr(name, shape, dt):
        kind = "ExternalOutput" if name in dbg else "Internal"
        return nc.dram_tensor(name, list(shape), dt, kind=kind).ap()

    x_in = din("x", [T, D])
    p_in = din("p", [NL, T, PLE])
    w_in = din("w_in", [NL, D, NIN])
    spT = din("spT", [NL, 128, 4, 128])
    sbias = din("sbias", [NL, 128, 4, 128])
    convw = din("convw", [NL, 128, 3, 4])
    poolw = din("poolw", [NL, 128, 4, 128])
    pscale = din("pscale", [NL, 128, 4])
    w_branch = din("w_branch", [NL, 4, 512, D])
    w_gate = din("w_gate", [NL, 4, D, D])
    w_out = din("w_out", [NL, D, D])
    ln1g = din("ln1g", [NL, 128, D])
    ln1b = din("ln1b", [NL, 128, D])
    router_w = din("router_w", [NL, D, E])
    router_b = din("router_b", [NL, 128, E])
    w_gu = din("w_gu", [NL * E * D, 2 * FF])
    b_gu = din("b_gu", [NL * E * 128, 16])
    w_dn = din("w_dn", [NL * E * FF, D])
    b_dn = din("b_dn", [NL * E, D])
    w_pg = din("w_pg", [NL, D, D])
    w_pp = din("w_pp", [NL, PLE, D])
    ln2g = din("ln2g", [NL, 128, D])
    ln2b = din("ln2b", [NL, 128, D])
    identf = din("identf", [128, 128])
    abias = din("abias", [128, 4 * 3 * 4 * 256])
    pcorr = din("pcorr", [128, 2 * 4 * 8])
    dbgT = {}
    if "BED" in dbg:
        dbgT["BED"] = nc.dram_tensor("BED", [1, NB], I32, kind="ExternalOutput").ap()
    for nm in ("YA2", "YB", "YC", "YD"):
        if "YD" in dbg:
            dbgT[nm] = nc.dram_tensor(nm, [4, 128, T], BF16, kind="ExternalOutput").ap()

    tril = din("tril", [128, 128])
    y_out = nc.dram_tensor("y", [T, D], F32, kind="ExternalOutput").ap()

    QKV = dscr("QKV", [9, T, 512], BF16)
    YA = dscr("YA", [4, 128, T], BF16)
    BB = dscr("BB", [4, 128, T], BF16)
    ZZ = dscr("ZZ", [4, 128, T], BF16)
    CZ = dscr("CZ", [4, 128, T], BF16)
    OO = dscr("OO", [3, T, 512], BF16)
    LSE = dscr("LSE", [3, T, 4], F32)
    MG = dscr("MG", [8, 128, T], BF16)
    X1 = dscr("X1", [T, D], F32)
    X1B = dscr("X1B", [T, D], BF16)
    X1T = dscr("X1T", [8, 128, T], BF16)
    XS = dscr("XS", [NSLOT, D], BF16)
    YSH = [dscr("YSa", [NSLOT, 512], F32), dscr("YSb", [NSLOT, 512], F32)]
    XN = dscr("XN", [T, D], F32)
    LGD = dscr("LGD", [T, 32], F32)
    DESTD = dscr("DESTD", [T, 4], I32)
    PROBD = dscr("PROBD", [T, 4], F32)

    with contextlib.ExitStack() as st:
        k = K(nc, st)
        ps = [k.psum(f"ps{i}", [128, 512], F32) for i in range(8)]
        psi = [0]

        def nps():
            b = ps[psi[0] % 8]
            psi[0] += 1
            return b

        flip = [0]

        def evq():
            flip[0] += 1
            return "act" if flip[0] % 2 else "dve"

        def copy(e, out, in_, reads, writes, fresh=None):
            if e == "act":
                return k.op("act", lambda g: g.activation(out=out, in_=in_, func=AF.Copy), reads, writes, fresh)
            return k.op(e, lambda g: g.tensor_copy(out=out, in_=in_), reads, writes, fresh)

        identb = k.sbuf("identb", [128, 128], BF16)
        k.dma("pool", identb[:], identf, wbuf=identb)
        identF = k.sbuf("identF", [128, 128], F32)
        k.dma("sp", identF[:], identf, wbuf=identF)

        def phase1(l, xsrc):
            wa = k.sbuf("w1", [128, 8 * NIN], BF16)
            k1_spT = k.sbuf("k1_spT", [128, 4, 128], BF16)
            k1_sbias = k.sbuf("k1_sbias", [128, 4, 128], F32)
            r_xb = Ring(k, "xb", 1, [128, 4, 1024], BF16)
            r_xT = Ring(k, "xT", 2, [128, 8, 512], BF16)
            r_auT = Ring(k, "auT", 2, [128, 4, 512], BF16)
            r_fm = Ring(k, "fm", 4, [128, 4, 512], BF16)
            r_st = Ring(k, "st", 4, [128, 16], F32)
            r_vn = Ring(k, "vn", 2, [128, 512], BF16)
            r_tmp = Ring(k, "tmp", 2, [128, 512], F32)
            r_qkv = Ring(k, "qkvs", 2, [128, 3, 512], BF16)
            w_sb = wa[:, 0:8 * NIN].rearrange("p (c n) -> p c n", c=8)
            for c in range(8):
                for h in range(4):
                    k.dma("pool", w_sb[:, c, h * 1920:(h + 1) * 1920], w_in[l, c * 128:(c + 1) * 128, h * 1920:(h + 1) * 1920],
                          wbuf=wa, fresh=(c == 0 and h == 0))
            spT_sb = k1_spT
            k.dma("pool", spT_sb[:], spT[l], wbuf=spT_sb)
            k.dma("sp", k1_sbias[:], sbias[l], wbuf=k1_sbias)
            for i in range(NT):
                t0 = i * 512
                xb = r_xb.next()
                k.dma("pool", xb[:], xsrc[t0:t0 + 512, :].rearrange("(s p) f -> p s f", p=128), wbuf=xb)
                xT = r_xT.next()
                for s in range(4):
                    pt = nps()
                    ptb = pt[:].bitcast(BF16)
                    for c in range(8):
                        k.op("pe", lambda g: g.transpose(out=ptb[:, c * 128:(c + 1) * 128], in_=xb[:, s, c * 128:(c + 1) * 128],
                                                         identity=identb[:]),
                             [xb, identb], [pt], [pt] if c == 0 else [], inc=(c == 7))
                    copy(evq(), xT[:, :, s * 128:(s + 1) * 128], ptb.rearrange("p (c t) -> p c t", c=8), [pt], [xT],
                         [xT] if s == 0 else [])

                def mm_fm(fb):
                    pm = nps()
                    for c in range(8):
                        k.op("pe", lambda g: g.matmul(pm[:], lhsT=w_sb[:, c, fb * 128:(fb + 1) * 128], rhs=xT[:, c, :],
                                                      start=(c == 0), stop=(c == 7)),
                             [wa, xT], [pm], [pm] if c == 0 else [], inc=(c == 7))
                    return pm

                def mm_tm(s, col0):
                    pm = nps()
                    for c in range(8):
                        k.op("pe", lambda g: g.matmul(pm[:], lhsT=xT[:, c, s * 128:(s + 1) * 128], rhs=w_sb[:, c, col0:col0 + 512],
                                                      start=(c == 0), stop=(c == 7)),
                             [wa, xT], [pm], [pm] if c == 0 else [], inc=(c == 7))
                    return pm

                auT = r_auT.next()
                for fb in range(4):
                    pm = mm_fm(fb)
                    copy("act", auT[:, fb, :], pm[:], [pm], [auT], [auT] if fb == 0 else [])
                yA = r_fm.next()
                for s in range(4):
                    pv = mm_tm(s, 512)
                    stt = r_st.next()
                    k.op("dve", lambda g: g.bn_stats(out=stt[:, 0:6], in_=pv[:]), [pv], [stt])
                    k.op("dve", lambda g: g.bn_aggr(out=stt[:, 6:8], in_=stt[:, 0:6]), [stt], [stt], [])
                    k.op("act", lambda g: g.activation(out=stt[:, 8:9], in_=stt[:, 7:8], func=AF.Sqrt, bias=k1_eps[:, 0:1], scale=1.0),
                         [stt, k1_eps], [stt], [])
                    k.op("dve", lambda g: g.reciprocal(out=stt[:, 9:10], in_=stt[:, 8:9]), [stt], [stt], [])
                    vn = r_vn.next()
                    k.op("dve", lambda g: g.tensor_scalar(out=vn[:], in0=pv[:], scalar1=stt[:, 6:7], scalar2=stt[:, 9:10],
                                                          op0=ALU.subtract, op1=ALU.mult), [pv, stt], [vn])
                    pm = nps()
                    for gq in range(4):
                        k.op("pe", lambda g: g.matmul(pm[:, gq * 128:(gq + 1) * 128], lhsT=vn[:, gq * 128:(gq + 1) * 128],
                                                      rhs=spT_sb[:, gq, :], start=True, stop=True),
                             [vn, spT_sb], [pm], [pm] if gq == 0 else [], inc=(gq == 3))
                    tmp = r_tmp.next()
                    k.op("dve", lambda g: g.tensor_tensor(out=tmp[:], in0=pm[:], in1=k1_sbias[:].rearrange("p g t -> p (g t)"),
                                                          op=ALU.add), [pm, k1_sbias], [tmp])
                    k.op("pool", lambda g: g.tensor_tensor(out=yA[:, :, s * 128:(s + 1) * 128], in0=auT[:, :, s * 128:(s + 1) * 128],
                                                           in1=tmp[:].rearrange("p (g t) -> p g t", g=4), op=ALU.mult),
                         [auT, tmp], [yA], [yA] if s == 0 else [])
                k.dma("sp", YA[:, :, t0:t0 + 512].rearrange("g p t -> p g t"), yA[:], rbuf=yA)
                bh = r_auT.next()
                for j in range(4):
                    pm = mm_fm(8 + j)
                    copy("act", bh[:, j, :], pm[:], [pm], [bh], [bh] if j == 0 else [])
                zz = r_fm.next()
                for j in range(4):
                    pm = mm_fm(16 + j)
                    k.op("dve", lambda g: g.tensor_tensor(out=zz[:, j, :], in0=pm[:], in1=bh[:, j, :], op=ALU.mult),
                         [pm, bh], [zz], [zz] if j == 0 else [])
                k.dma("sp", ZZ[:, :, t0:t0 + 512].rearrange("g p t -> p g t"), zz[:], rbuf=zz)
                bb = r_fm.next()
                for j in range(4):
                    pm = mm_fm(12 + j)
                    copy(evq(), bb[:, j, :], pm[:], [pm], [bb], [bb] if j == 0 else [])
                k.dma("sp", BB[:, :, t0:t0 + 512].rearrange("g p t -> p g t"), bb[:], rbuf=bb)
                cz = r_fm.next()
                for j in range(4):
                    pm = mm_fm(20 + j)
                    copy(evq(), cz[:, j, :], pm[:], [pm], [cz], [cz] if j == 0 else [])
                k.dma("sp", CZ[:, :, t0:t0 + 512].rearrange("g p t -> p g t"), cz[:], rbuf=cz)
                for s in range(4):
                    r0 = t0 + s * 128
                    for j3 in range(3):
                        qs = r_qkv.next()
                        for jj in range(3):
                            pm = mm_tm(s, 3072 + (j3 * 3 + jj) * 512)
                            copy(evq(), qs[:, jj, :], pm[:], [pm], [qs], [qs] if jj == 0 else [])
                        k.dma("sp", QKV[j3 * 3:j3 * 3 + 3, r0:r0 + 128, :].rearrange("j t f -> t j f"), qs[:], rbuf=qs)

        def phaseA(l):
            ab = k.sbuf("abias", [128, 48 * 256], F32)
            for q4 in range(4):
                k.dma("sp", ab[:, q4 * 3072:(q4 + 1) * 3072], abias[:, q4 * 3072:(q4 + 1) * 3072], wbuf=ab, fresh=(q4 == 0))
            r_q = Ring(k, "aq", 2, [128, 8, 512], BF16)
            r_k = Ring(k, "ak", 2, [128, 9, 512], BF16)
            r_v = Ring(k, "av", 2, [128, 9, 512], BF16)
            r_qT = Ring(k, "aqT", 2, [128, 4, 1024], BF16)
            r_kT = Ring(k, "akT", 2, [128, 4, 1152], BF16)
            r_sb = Ring(k, "asb", 4, [128, 4, 256], F32)
            r_p = Ring(k, "ap", 4, [128, 4, 256], BF16)
            r_pt = Ring(k, "apt", 4, [128, 4, 2, 128], BF16)
            r_st = Ring(k, "ast", 8, [128, 32], F32)
            r_o = Ring(k, "ao", 2, [128, 8, 512], BF16)
            r_l = Ring(k, "al", 2, [128, 8, 4], F32)
            pS = [(ps[0], ps[1]), (ps[2], ps[3])]
            pT = [ps[4], ps[5]]
            pU = [ps[6], ps[7]]
            cnt = [0]

            def unit(seq_off, S, g, d, r, a, b):
                L = S // d
                ntile = L // 128
                nq = b - a

                def sub(t2d):
                    return t2d[seq_off:seq_off + S, :].rearrange("(i d) f -> d i f", d=d)[r]
                qd, kd, vd = sub(QKV[3 * g]), sub(QKV[3 * g + 1]), sub(QKV[3 * g + 2])
                qb = r_q.next()
                k.dma("sp", qb[:, 0:nq, :], qd[128 * a:128 * b, :].rearrange("(m p) f -> p m f", p=128), wbuf=qb)
                kb, vb = r_k.next(), r_v.next()
                for (buf, src, q) in ((kb, kd, "sp"), (vb, vd, "act")):
                    first = True
                    m_lo, m_hi = max(a, 1), min(b, ntile - 1)
                    if m_lo <= m_hi:
                        k.dma(q, buf[:, m_lo - a:m_hi - a + 1, :],
                              src[128 * m_lo - 64:128 * m_hi + 64, :].rearrange("(m p) f -> p m f", p=128), wbuf=buf, fresh=first)
                        first = False
                    if a == 0:
                        k.dma(q, buf[64:128, 0, :], src[0:64, :], wbuf=buf, fresh=first)
                        first = False
                        k.op("pool", lambda e: e.memset(buf[0:64, 0, :], 0.0), [], [buf], [])
                    if b == ntile:
                        k.dma(q, buf[0:64, ntile - a, :], src[L - 64:L, :], wbuf=buf, fresh=first)
                        first = False
                        k.op("pool", lambda e: e.memset(buf[64:128, ntile - a, :], 0.0), [], [buf], [])
                qT, kT = r_qT.next(), r_kT.next()
                for (src, dst, n) in ((qb, qT, nq), (kb, kT, nq + 1)):
                    j = 0
                    while j < n:
                        w = min(2, n - j)
                        pt = pT[cnt[0] % 2]
                        cnt[0] += 1
                        ptb = pt[:].bitcast(BF16)
                        for h in range(4):
                            for jj in range(w):
                                last = (h == 3 and jj == w - 1)
                                k.op("pe", lambda e: e.transpose(out=ptb[:, (h * w + jj) * 128:(h * w + jj + 1) * 128],
                                                                 in_=src[:, j + jj, h * 128:(h + 1) * 128], identity=identb[:]),
                                     [src, identb], [pt], [pt] if (h == 0 and jj == 0) else [], inc=last)
                        copy(evq(), dst[:, :, j * 128:(j + w) * 128], ptb[:, 0:4 * w * 128].rearrange("p (h x) -> p h x", h=4),
                             [pt], [dst], [dst] if j == 0 else [])
                        j += w
                ost, lst = r_o.next(), r_l.next()

                def stageA(j):
                    pa_, pb_ = pS[j % 2]
                    for h in range(4):
                        pp = pa_ if h < 2 else pb_
                        hh = h % 2
                        k.op("pe", lambda e: e.matmul(pp[:, hh * 256:(hh + 1) * 256], lhsT=qT[:, h, j * 128:(j + 1) * 128],
                                                      rhs=kT[:, h, j * 128:j * 128 + 256], start=True, stop=True),
                             [qT, kT], [pp], [pp] if hh == 0 else [], inc=(hh == 1))

                stt_ = {}
                sb_ = {}
                pb_d = {}
                ptu = {}

                def stageB1(j):
                    ja = a + j
                    var = (1 if ja == 0 else 0) | (2 if ja == ntile - 1 else 0)
                    pa_, pb_ = pS[j % 2]
                    sb = r_sb.next()
                    sb_[j] = sb
                    for half, pp in enumerate((pa_, pb_)):
                        base = ((var * 3 + g) * 4 + half * 2) * 256
                        k.op("dve", lambda e: e.scalar_tensor_tensor(out=sb[:, half * 2:half * 2 + 2, :].rearrange("p h x -> p (h x)"),
                                                                     in0=pp[:], scalar=SCALE, in1=ab[:, base:base + 512],
                                                                     op0=ALU.mult, op1=ALU.add),
                             [pp, ab], [sb], [sb] if half == 0 else [])
                    stt = r_st.next()
                    stt_[j] = stt
                    k.op("dve", lambda e: e.tensor_reduce(out=stt[:, 0:4], in_=sb[:], axis=AX.X, op=ALU.max), [sb], [stt])
                    k.op("dve", lambda e: e.tensor_scalar(out=stt[:, 4:8], in0=stt[:, 0:4], scalar1=-1.0, scalar2=None, op0=ALU.mult),
                         [stt], [stt], [])

                def stageB2(j):
                    sb, stt = sb_[j], stt_[j]
                    pb = r_p.next()
                    pb_d[j] = pb
                    for h in range(4):
                        k.op("act", lambda e: e.activation(out=pb[:, h, :], in_=sb[:, h, :], func=AF.Exp, bias=stt[:, 4 + h:5 + h],
                                                           scale=1.0, accum_out=stt[:, 8 + h:9 + h]),
                             [sb, stt], [pb, stt], [pb] if h == 0 else [])

                def stageB3(j):
                    pb = pb_d[j]
                    pt = pT[cnt[0] % 2]
                    pu = pU[cnt[0] % 2]
                    cnt[0] += 1
                    ptb = pt[:].bitcast(BF16)
                    for h in range(4):
                        for c in range(2):
                            k.op("pe", lambda e: e.transpose(out=ptb[:, (h * 2 + c) * 128:(h * 2 + c + 1) * 128],
                                                             in_=pb[:, h, c * 128:(c + 1) * 128], identity=identb[:]),
                                 [pb, identb], [pt], [pt] if (h == 0 and c == 0) else [], inc=(h == 3 and c == 1))
                    ptt = r_pt.next()
                    copy("act", ptt[:].rearrange("p h c t -> p (h c t)"), ptb, [pt], [ptt])
                    ptu[j] = (ptt, pu)

                def stagePV(j):
                    ptt, pu = ptu[j]
                    for h in range(4):
                        for c in range(2):
                            k.op("pe", lambda e: e.matmul(pu[:, h * 128:(h + 1) * 128], lhsT=ptt[:, h, c, :],
                                                          rhs=vb[:, j + c, h * 128:(h + 1) * 128], start=(c == 0), stop=(c == 1)),
                                 [ptt, vb], [pu], [pu] if (h == 0 and c == 0) else [], inc=(h == 3 and c == 1))

                def tail1(j):
                    stt = stt_[j]
                    k.op("dve", lambda e: e.reciprocal(out=stt[:, 12:16], in_=stt[:, 8:12]), [stt], [stt], [])

                def tail2(j):
                    stt = stt_[j]
                    ptt, pu = ptu[j]
                    for h in range(4):
                        k.op("act", lambda e: e.activation(out=ost[:, j, h * 128:(h + 1) * 128], in_=pu[:, h * 128:(h + 1) * 128],
                                                           func=AF.Copy, scale=stt[:, 12 + h:13 + h]),
                             [pu, stt], [ost], [ost] if (j == 0 and h == 0) else [])
                    k.op("act", lambda e: e.activation(out=stt[:, 16:20], in_=stt[:, 8:12], func=AF.Ln), [stt], [stt], [])

                def tail3(j):
                    stt = stt_[j]
                    k.op("dve", lambda e: e.tensor_tensor(out=lst[:, j, :], in0=stt[:, 16:20], in1=stt[:, 0:4], op=ALU.add),
                         [stt], [lst], [lst] if j == 0 else [])

                pairs = [list(range(j, min(j + 2, nq))) for j in range(0, nq, 2)]
                for j in pairs[0]:
                    stageA(j)
                prev = None
                for pi, pr in enumerate(pairs):
                    for j in pr:
                        stageB1(j)
                    if prev is not None:
                        for j in prev:
                            tail1(j)
                        for j in prev:
                            tail2(j)
                        for j in prev:
                            tail3(j)
                    for j in pr:
                        stageB2(j)
                    if pi + 1 < len(pairs):
                        for j in pairs[pi + 1]:
                            stageA(j)
                    for j in pr:
                        stageB3(j)
                    for j in pr:
                        stagePV(j)
                    prev = pr
                for j in prev:
                    tail1(j)
                for j in prev:
                    tail2(j)
                for j in prev:
                    tail3(j)
                od = sub(OO[g])
                k.dma("sp", od[128 * a:128 * b, :].rearrange("(m p) f -> p m f", p=128), ost[:, 0:nq, :], rbuf=ost)
                ld = sub(LSE[g])
                k.dma("act", ld[128 * a:128 * b, :].rearrange("(m p) f -> p m f", p=128), lst[:, 0:nq, :], rbuf=lst)

            off = 0
            for S in seqs:
                for g, (_, d) in enumerate(ATT):
                    L = S // d
                    ntile = L // 128
                    for r in range(d):
                        for a in range(0, ntile, 8):
                            unit(off, S, g, d, r, a, min(a + 8, ntile))
                off += S


        def load_xT(xsrc, t0, r_xb, r_xT):
            xb = r_xb.next()
            k.dma("pool", xb[:], xsrc[t0:t0 + 512, :].rearrange("(s p) f -> p s f", p=128), wbuf=xb)
            xT = r_xT.next()
            for s in range(4):
                pt = nps()
                ptb = pt[:].bitcast(BF16)
                for c in range(8):
                    k.op("pe", lambda g: g.transpose(out=ptb[:, c * 128:(c + 1) * 128], in_=xb[:, s, c * 128:(c + 1) * 128],
                                                     identity=identb[:]),
                         [xb, identb], [pt], [pt] if c == 0 else [], inc=(c == 7))
                copy(evq(), xT[:, :, s * 128:(s + 1) * 128], ptb.rearrange("p (c t) -> p c t", c=8), [pt], [xT],
                     [xT] if s == 0 else [])
            return xT

        def seq_pos():
            off = 0
            for S in seqs:
                for i in range(S // 512):
                    yield off + i * 512, i == 0, i == S // 512 - 1
                off += S

        def phase2a(l, xsrc):
            wg = k.sbuf("wg", [128, 4, 8, 1024], BF16)
            wbr = k.sbuf("wbr", [128, 4, 4, 1024], BF16)
            for i in range(4):
                for c in range(8):
                    k.dma("pool", wg[:, i, c, :], w_gate[l, i, c * 128:(c + 1) * 128, :], wbuf=wg, fresh=(i == 0 and c == 0))
                for c in range(4):
                    k.dma("pool", wbr[:, i, c, :], w_branch[l, i, c * 128:(c + 1) * 128, :], wbuf=wbr, fresh=(i == 0 and c == 0))
            cw = k.sbuf("cw", [128, 3, 4], F32)
            k.dma("sp", cw[:], convw[l], wbuf=cw)
            pw = k.sbuf("pw", [128, 4, 128], BF16)
            k.dma("pool", pw[:], poolw[l], wbuf=pw)
            psc = k.sbuf("psc", [128, 4], F32)
            k.dma("sp", psc[:], pscale[l], wbuf=psc)
            pc = k.sbuf("pcorr", [128, 2, 4, 8], F32)
            k.dma("sp", pc[:].rearrange("p a g t -> p (a g t)"), pcorr, wbuf=pc)
            r_xb = Ring(k, "xb", 1, [128, 4, 1024], BF16)
            r_xT = Ring(k, "xT", 1, [128, 8, 512], BF16)
            r_y = [Ring(k, f"y{i}", 1, [128, 4, 512], BF16) for i in range(4)]
            r_bb = Ring(k, "bb", 1, [128, 4, 512], BF16)
            r_zh = Ring(k, "zh", 1, [128, 4, 514], BF16)
            r_czh = Ring(k, "czh", 1, [128, 4, 528], BF16)
            r_og = Ring(k, "og", 2, [128, 3, 512], BF16)
            r_ls = Ring(k, "ls", 2, [128, 4, 3, 4], F32)
            r_t = Ring(k, "t2", 5, [128, 528], F32)
            r_pl = Ring(k, "pl", 2, [128, 512], BF16)
            r_sm = Ring(k, "sm", 4, [128, 64], F32)
            r_acc = Ring(k, "acc", 2, [128, 512], F32)
            r_ydt = Ring(k, "ydt", 2, [128, 512], BF16)
            r_sg = Ring(k, "sg", 3, [128, 512], BF16)
            r_ma = Ring(k, "ma", 2, [128, 512], F32)
            r_mt = Ring(k, "mt", 2, [128, 512], F32)
            r_mg = Ring(k, "mg", 3, [128, 512], BF16)
            for (t0, first, last) in seq_pos():
                xT = load_xT(xsrc, t0, r_xb, r_xT)
                yA = r_y[0].next()
                k.dma("sp", yA[:], YA[:, :, t0:t0 + 512].rearrange("g p t -> p g t"), wbuf=yA)
                bb = r_bb.next()
                k.dma("sp", bb[:], BB[:, :, t0:t0 + 512].rearrange("g p t -> p g t"), wbuf=bb)
                zh = r_zh.next()
                lo, hi = (1 if first else 0), (513 if last else 514)
                k.dma("act", zh[:, :, lo:hi], ZZ[:, :, t0 - 1 + lo:t0 - 1 + hi].rearrange("g p t -> p g t"), wbuf=zh)
                if first:
                    k.op("pool", lambda e: e.memset(zh[:, :, 0:1], 0.0), [], [zh], [])
                if last:
                    k.op("pool", lambda e: e.memset(zh[:, :, 513:514], 0.0), [], [zh], [])
                czh = r_czh.next()
                lo, hi = (8 if first else 0), (520 if last else 528)
                k.dma("act", czh[:, :, lo:hi], CZ[:, :, t0 - 8 + lo:t0 - 8 + hi].rearrange("g p t -> p g t"), wbuf=czh)
                if first:
                    k.op("pool", lambda e: e.memset(czh[:, :, 0:8], 0.0), [], [czh], [])
                if last:
                    k.op("pool", lambda e: e.memset(czh[:, :, 520:528], 0.0), [], [czh], [])
                ls = r_ls.next()
                for g in range(3):
                    k.dma("act", ls[:, :, g, :], LSE[g, t0:t0 + 512, :].rearrange("(s p) h -> p s h", p=128), wbuf=ls, fresh=(g == 0))
                yB = r_y[1].next()
                for fb in range(4):
                    tb = r_t.next()
                    k.op("dve", lambda e: e.tensor_scalar(out=tb[:, 0:512], in0=zh[:, fb, 0:512], scalar1=cw[:, 0, fb:fb + 1], scalar2=None,
                                                          op0=ALU.mult), [zh, cw], [tb])
                    for j in (1, 2):
                        k.op("dve", lambda e: e.scalar_tensor_tensor(out=tb[:, 0:512], in0=zh[:, fb, j:j + 512], scalar=cw[:, j, fb:fb + 1],
                                                                     in1=tb[:, 0:512], op0=ALU.mult, op1=ALU.add), [zh, cw, tb], [tb], [])
                    k.op("dve", lambda e: e.tensor_tensor(out=yB[:, fb, :], in0=tb[:, 0:512], in1=bb[:, fb, :], op=ALU.mult),
                         [tb, bb], [yB], [yB] if fb == 0 else [])
                yC = r_y[2].next()
                for g in range(4):
                    w = POOLW[g]
                    cur = r_t.next()
                    k.op("pool", lambda e: e.tensor_tensor(out=cur[:, 1:528], in0=czh[:, g, 0:527], in1=czh[:, g, 1:528], op=ALU.add),
                         [czh], [cur])
                    half = 1
                    lo_, hi_ = 1, 528
                    while half * 2 < w:
                        nxt = r_t.next()
                        nlo, nhi = lo_ + half, hi_ - half
                        k.op("pool", lambda e: e.tensor_tensor(out=nxt[:, nlo:nhi], in0=cur[:, nlo - half:nhi - half],
                                                               in1=cur[:, nlo + half:nhi + half], op=ALU.add), [cur], [nxt])
                        cur, lo_, hi_, half = nxt, nlo, nhi, half * 2
                    if first:
                        k.op("dve", lambda e: e.tensor_tensor(out=cur[:, 8:16], in0=cur[:, 8:16], in1=pc[:, 0, g, :], op=ALU.mult),
                             [cur, pc], [cur], [])
                    if last:
                        k.op("dve", lambda e: e.tensor_tensor(out=cur[:, 512:520], in0=cur[:, 512:520], in1=pc[:, 1, g, :], op=ALU.mult),
                             [cur, pc], [cur], [])
                    pl = r_pl.next()
                    k.op("dve", lambda e: e.scalar_tensor_tensor(out=pl[:], in0=cur[:, 8:520], scalar=1.0 / w, in1=czh[:, g, 8:520],
                                                                 op0=ALU.mult, op1=ALU.subtract), [cur, czh], [pl])
                    pm = nps()
                    k.op("pe", lambda e: e.matmul(pm[:], lhsT=pw[:, g, :], rhs=pl[:], start=True, stop=True), [pw, pl], [pm])
                    k.op("act", lambda e: e.activation(out=yC[:, g, :], in_=pm[:], func=AF.Copy, scale=psc[:, g:g + 1]),
                         [pm, psc], [yC], [yC] if g == 0 else [])
                yD = r_y[3].next()
                for s in range(4):
                    og = r_og.next()
                    k.dma("sp", og[:], OO[:, t0 + s * 128:t0 + (s + 1) * 128, :].rearrange("g p f -> p g f"), wbuf=og)
                    sm = r_sm.next()
                    lsv = ls[:, s, :, :]
                    k.op("dve", lambda e: e.tensor_tensor(out=sm[:, 0:4], in0=lsv[:, 0, :], in1=lsv[:, 1, :], op=ALU.max), [ls], [sm])
                    k.op("dve", lambda e: e.tensor_tensor(out=sm[:, 0:4], in0=sm[:, 0:4], in1=lsv[:, 2, :], op=ALU.max), [ls, sm], [sm], [])
                    for g in range(3):
                        k.op("dve", lambda e: e.tensor_tensor(out=sm[:, 4 + 4 * g:8 + 4 * g], in0=lsv[:, g, :], in1=sm[:, 0:4],
                                                              op=ALU.subtract), [ls, sm], [sm], [])
                    k.op("act", lambda e: e.activation(out=sm[:, 16:28], in_=sm[:, 4:16], func=AF.Exp), [sm], [sm], [])
                    k.op("dve", lambda e: e.tensor_tensor(out=sm[:, 28:32], in0=sm[:, 16:20], in1=sm[:, 20:24], op=ALU.add), [sm], [sm], [])
                    k.op("dve", lambda e: e.tensor_tensor(out=sm[:, 28:32], in0=sm[:, 28:32], in1=sm[:, 24:28], op=ALU.add), [sm], [sm], [])
                    k.op("dve", lambda e: e.reciprocal(out=sm[:, 32:36], in_=sm[:, 28:32]), [sm], [sm], [])
                    for g in range(3):
                        k.op("dve", lambda e: e.tensor_tensor(out=sm[:, 36 + 4 * g:40 + 4 * g], in0=sm[:, 16 + 4 * g:20 + 4 * g],
                                                              in1=sm[:, 32:36], op=ALU.mult), [sm], [sm], [])
                    acc = r_acc.next()
                    for h in range(4):
                        hs = slice(h * 128, (h + 1) * 128)
                        k.op("dve", lambda e: e.tensor_scalar(out=acc[:, hs], in0=og[:, 0, hs], scalar1=sm[:, 36 + h:37 + h], scalar2=None,
                                                              op0=ALU.mult), [og, sm], [acc], [acc] if h == 0 else [])
                        for g in (1, 2):
                            last_ = (g == 2)
                            k.op("dve", lambda e: e.scalar_tensor_tensor(out=acc[:, hs], in0=og[:, g, hs],
                                                                         scalar=sm[:, 36 + 4 * g + h:37 + 4 * g + h], in1=acc[:, hs],
                                                                         op0=ALU.mult, op1=ALU.add), [og, sm, acc], [acc], [])
                    ydt = r_ydt.next()
                    copy("act", ydt[:], acc[:], [acc], [ydt])
                    pt = nps()
                    ptb = pt[:].bitcast(BF16)
                    for h in range(4):
                        k.op("pe", lambda e: e.transpose(out=ptb[:, h * 128:(h + 1) * 128], in_=ydt[:, h * 128:(h + 1) * 128], identity=identb[:]),
                             [ydt, identb], [pt], [pt] if h == 0 else [], inc=(h == 3))
                    copy(evq(), yD[:, :, s * 128:(s + 1) * 128], ptb[:, 0:512].rearrange("p (h t) -> p h t", h=4), [pt], [yD],
                         [yD] if s == 0 else [])
                ys = (yA, yB, yC, yD)
                if "YD" in dbg:
                    for i_, nm in enumerate(("YA2", "YB", "YC", "YD")):
                        k.dma("sp", dbgT[nm][:, :, t0:t0 + 512].rearrange("g p t -> p g t"), ys[i_][:], rbuf=ys[i_])
                for fb in range(8):
                    fs = slice(fb * 128, (fb + 1) * 128)
                    ma = r_ma.next()
                    mgo = r_mg.next()
                    for i in range(4):
                        pg = nps()
                        for c in range(8):
                            k.op("pe", lambda e: e.matmul(pg[:], lhsT=wg[:, i, c, fs], rhs=xT[:, c, :], start=(c == 0), stop=(c == 7)),
                                 [wg, xT], [pg], [pg] if c == 0 else [], inc=(c == 7))
                        sg = r_sg.next()
                        k.op("act", lambda e: e.activation(out=sg[:], in_=pg[:], func=AF.Sigmoid), [pg], [sg])
                        pp = nps()
                        for c in range(4):
                            k.op("pe", lambda e: e.matmul(pp[:], lhsT=wbr[:, i, c, fs], rhs=ys[i][:, c, :], start=(c == 0), stop=(c == 3)),
                                 [wbr, ys[i]], [pp], [pp] if c == 0 else [], inc=(c == 3))
                        if i == 0:
                            k.op("dve", lambda e: e.tensor_tensor(out=ma[:], in0=pp[:], in1=sg[:], op=ALU.mult), [pp, sg], [ma])
                        else:
                            mt = r_mt.next()
                            k.op("dve", lambda e: e.tensor_tensor(out=mt[:], in0=pp[:], in1=sg[:], op=ALU.mult), [pp, sg], [mt])
                            if i < 3:
                                k.op("pool", lambda e: e.tensor_tensor(out=ma[:], in0=ma[:], in1=mt[:], op=ALU.add), [ma, mt], [ma], [])
                            else:
                                k.op("pool", lambda e: e.tensor_tensor(out=mgo[:], in0=ma[:], in1=mt[:], op=ALU.add), [ma, mt], [mgo])
                    k.dma("sp", MG[fb, :, t0:t0 + 512], mgo[:], rbuf=mgo)

        def phase2b(l, xsrc):
            wo = k.sbuf("wo", [128, 8, 1024], BF16)
            for c in range(8):
                k.dma("pool", wo[:, c, :], w_out[l, c * 128:(c + 1) * 128, :], wbuf=wo, fresh=(c == 0))
            lg_, lb_ = k.sbuf("ln1g", [128, 1024], F32), k.sbuf("ln1b", [128, 1024], F32)
            k.dma("sp", lg_[:], ln1g[l], wbuf=lg_)
            k.dma("sp", lb_[:], ln1b[l], wbuf=lb_)
            rw = k.sbuf("rw", [128, 8, 32], F32)
            k.dma("sp", rw[:], router_w[l].rearrange("(c p) e -> p c e", p=128), wbuf=rw)
            rwh = k.sbuf("rwh", [128, 8, 32], BF16)
            k.dma("pool", rwh[:], router_w[l].rearrange("(c p) e -> p c e", p=128), wbuf=rwh)
            rwl = k.sbuf("rwl", [128, 8, 32], BF16)
            k.op("dve", lambda e: e.tensor_tensor(out=rwl[:], in0=rw[:], in1=rwh[:], op=ALU.subtract), [rw, rwh], [rwl])
            rb = k.sbuf("rb", [128, 32], F32)
            k.dma("sp", rb[:], router_b[l], wbuf=rb)
            r_mg = Ring(k, "mgl", 2, [128, 8, 512], BF16)
            r_x = Ring(k, "xf", 2, [128, 4, 1024], F32)
            r_rr = Ring(k, "rr", 2, [128, 1024], F32)
            r_st = Ring(k, "st", 4, [128, 32], F32)
            r_x1 = Ring(k, "x1", 2, [128, 1024], F32)
            r_x1b = Ring(k, "x1b", 2, [128, 1024], BF16)
            r_x1l = Ring(k, "x1l", 2, [128, 1024], BF16)
            r_x1Tl = Ring(k, "x1Tl", 2, [128, 8, 128], BF16)
            r_x1Tb = Ring(k, "x1Tb", 2, [128, 8, 512], BF16)
            r_lg = Ring(k, "lgs", 2, [128, 4, 32], F32)
            zt = k.sbuf("zt", [128, 4096], BF16)
            k.op("pool", lambda e: e.memset(zt[:], 0.0), [], [zt])
            nz = NSLOT // 512
            zper = -(-nz // NT)
            for i in range(NT):
                t0 = i * 512
                mg = r_mg.next()
                k.dma("sp", mg[:], MG[:, :, t0:t0 + 512].rearrange("c p t -> p c t"), wbuf=mg)
                xf = r_x.next()
                k.dma("act", xf[:], xsrc[t0:t0 + 512, :].rearrange("(s p) f -> p s f", p=128), wbuf=xf)
                for b in range(i * zper, min((i + 1) * zper, nz)):
                    k.dma("act", XS[b * 512:(b + 1) * 512, :].rearrange("(p s) f -> p (s f)", s=4), zt[:], rbuf=zt)
                x1Tb = r_x1Tb.next()
                lgs = r_lg.next()
                for s in range(4):
                    rr = r_rr.next()
                    for half in range(2):
                        pw_ = nps()
                        for c in range(8):
                            k.op("pe", lambda e: e.matmul(pw_[:], lhsT=mg[:, c, s * 128:(s + 1) * 128], rhs=wo[:, c, half * 512:(half + 1) * 512],
                                                          start=(c == 0), stop=(c == 7)), [mg, wo], [pw_], [pw_] if c == 0 else [], inc=(c == 7))
                        k.op("dve", lambda e: e.scalar_tensor_tensor(out=rr[:, half * 512:(half + 1) * 512], in0=xf[:, s, half * 512:(half + 1) * 512],
                                                                     scalar=ALPHA, in1=pw_[:], op0=ALU.mult, op1=ALU.add),
                             [xf, pw_], [rr], [rr] if half == 0 else [])
                    x1 = r_x1.next()
                    layer_norm_tm(rr, x1, lg_, lb_, r_st)
                    r0 = t0 + s * 128
                    k.dma("sp", X1[r0:r0 + 128, :], x1[:], rbuf=x1)
                    x1b = r_x1b.next()
                    copy("act", x1b[:], x1[:], [x1], [x1b])
                    k.dma("act", X1B[r0:r0 + 128, :], x1b[:], rbuf=x1b)
                    x1l = r_x1l.next()
                    k.op("dve", lambda e: e.tensor_tensor(out=x1l[:], in0=x1[:], in1=x1b[:], op=ALU.subtract), [x1, x1b], [x1l])
                    x1Tl = r_x1Tl.next()
                    for (src, hi) in ((x1b, True), (x1l, False)):
                        pt = nps()
                        ptb = pt[:].bitcast(BF16)
                        for c in range(8):
                            k.op("pe", lambda e: e.transpose(out=ptb[:, c * 128:(c + 1) * 128], in_=src[:, c * 128:(c + 1) * 128], identity=identb[:]),
                                 [src, identb], [pt], [pt] if c == 0 else [], inc=(c == 7))
                        if hi:
                            copy("act", x1Tb[:, :, s * 128:(s + 1) * 128], ptb.rearrange("p (c t) -> p c t", c=8), [pt], [x1Tb],
                                 [x1Tb] if s == 0 else [])
                        else:
                            copy("dve", x1Tl[:], ptb.rearrange("p (c t) -> p c t", c=8), [pt], [x1Tl])
                    pl = nps()
                    n3 = 0
                    for (xa, wa_) in ((0, rwh), (1, rwh), (0, rwl)):
                        for c in range(8):
                            lhs = x1Tb[:, c, s * 128:(s + 1) * 128] if xa == 0 else x1Tl[:, c, :]
                            k.op("pe", lambda e: e.matmul(pl[:, 0:32], lhsT=lhs, rhs=wa_[:, c, :], start=(n3 == 0), stop=(n3 == 23)),
                                 [x1Tb, x1Tl, wa_], [pl], [pl] if n3 == 0 else [], inc=(n3 == 23))
                            n3 += 1
                    k.op("dve", lambda e: e.tensor_tensor(out=lgs[:, s, :], in0=pl[:, 0:32], in1=rb[:], op=ALU.add), [pl, rb], [lgs],
                         [lgs] if s == 0 else [])
                k.dma("sp", X1T[:, :, t0:t0 + 512].rearrange("c p t -> p c t"), x1Tb[:], rbuf=x1Tb)
                k.dma("act", LGD[t0:t0 + 512, :].rearrange("(s p) e -> p s e", p=128), lgs[:], rbuf=lgs)

        def layer_norm_tm(rr, out, g_, b_, r_st, e1="pool", e2="pool"):
            stt = r_st.next()
            for half in range(2):
                k.op("dve", lambda e: e.bn_stats(out=stt[:, half * 6:half * 6 + 6], in_=rr[:, half * 512:(half + 1) * 512]), [rr], [stt],
                     [stt] if half == 0 else [])
            k.op("dve", lambda e: e.bn_aggr(out=stt[:, 12:14], in_=stt[:, 0:12]), [stt], [stt], [])
            k.op("act", lambda e: e.activation(out=stt[:, 14:15], in_=stt[:, 13:14], func=AF.Sqrt, bias=k1_eps[:, 0:1], scale=1.0),
                 [stt, k1_eps], [stt], [])
            k.op("dve", lambda e: e.reciprocal(out=stt[:, 15:16], in_=stt[:, 14:15]), [stt], [stt], [])
            k.op("dve", lambda e: e.tensor_scalar(out=rr[:], in0=rr[:], scalar1=stt[:, 12:13], scalar2=stt[:, 15:16],
                                                  op0=ALU.subtract, op1=ALU.mult), [rr, stt], [rr], [])
            k.op(e1, lambda e: e.tensor_tensor(out=rr[:], in0=rr[:], in1=g_[:], op=ALU.mult), [rr, g_], [rr], [])
            k.op(e2, lambda e: e.tensor_tensor(out=out[:], in0=rr[:], in1=b_[:], op=ALU.add), [rr, b_], [out])


        NTT = T // 128
        LOGBLK = BLK.bit_length() - 1
        NSUB = BLK // 128

        def phase3(l):
            DKi = k.sbuf("DKi", [128, NTT, 4], I32)
            GUI = k.sbuf("GUI", [128, NB, 8], I32)
            BGI = k.sbuf("BGI", [128, NB], I32)
            BDI = k.sbuf("BDI", [128, NB], I32)
            with k.scope():
                route(l, DKi, GUI, BGI, BDI)
            with k.scope():
                dispatch(DKi)
            with k.scope():
                experts(l, GUI, BGI, BDI)

        def route(l, DKi, GUI, BGI, BDI):
            LG = k.sbuf("LG", [128, NTT, 32], F32)
            k.dma("sp", LG[:], LGD.rearrange("(n p) e -> p n e", p=128), wbuf=LG)
            mx8 = k.sbuf("mx8", [128, NTT, 8], F32)
            for n in range(NTT):
                k.op("dve", lambda e: e.max(out=mx8[:, n, :], in_=LG[:, n, :]), [LG], [mx8], [mx8] if n == 0 else [])
            M = k.sbuf("Mk", [128, NTT, 32], BF16)
            k.op("dve", lambda e: e.tensor_tensor(out=M[:], in0=LG[:], in1=mx8[:, :, 3:4].to_broadcast([128, NTT, 32]), op=ALU.is_ge),
                 [LG, mx8], [M])
            trilb = k.sbuf("trilb", [128, 128], BF16)
            k.dma("pool", trilb[:], tril, wbuf=trilb)
            onesb = k.sbuf("onesb", [128, 128], BF16)
            k.op("dve", lambda e: e.memset(onesb[:], 1.0), [], [onesb])
            RK = k.sbuf("RK", [128, NTT, 32], F32)
            CT = k.sbuf("CT", [128, NTT, 32], F32)
            n0 = 0
            while n0 < NTT:
                w = min(16, NTT - n0)
                for (lhs, dst) in ((trilb, RK), (onesb, CT)):
                    pm = nps()
                    k.op("pe", lambda e: e.matmul(pm[:, 0:w * 32], lhsT=lhs[:], rhs=M[:, n0:n0 + w, :].rearrange("p n e -> p (n e)"),
                                                  start=True, stop=True), [lhs, M], [pm])
                    copy(evq(), dst[:, n0:n0 + w, :].rearrange("p n e -> p (n e)"), pm[:, 0:w * 32], [pm], [dst], [dst] if n0 == 0 else [])
                n0 += w
            PRE = k.sbuf("PRE", [128, NTT + 1, 32], F32)
            k.op("dve", lambda e: e.memset(PRE[:, 0, :], 0.0), [], [PRE])
            for n in range(NTT):
                k.op("dve", lambda e: e.tensor_tensor(out=PRE[:, n + 1, :], in0=PRE[:, n, :], in1=CT[:, n, :], op=ALU.add), [PRE, CT], [PRE], [])
            sm = k.sbuf("rsm", [128, 8, 32], F32)
            smi = k.sbuf("rsmi", [128, 2, 32], I32)
            TOT = PRE[:, NTT, :]
            k.op("dve", lambda e: e.tensor_scalar(out=smi[:, 0, :], in0=TOT, scalar1=float(BLK - 1), scalar2=None, op0=ALU.add), [PRE], [smi])
            k.op("dve", lambda e: e.tensor_scalar(out=smi[:, 1, :], in0=smi[:, 0, :], scalar1=LOGBLK, scalar2=None, op0=ALU.arith_shift_right),
                 [smi], [smi], [])
            k.op("dve", lambda e: e.tensor_scalar(out=smi[:, 0, :], in0=smi[:, 1, :], scalar1=LOGBLK, scalar2=None, op0=ALU.logical_shift_left),
                 [smi], [smi], [])
            PAD, ENDS, START, ONES = sm[:, 0, :], sm[:, 1, :], sm[:, 2, :], sm[:, 3, :]
            k.op("dve", lambda e: e.tensor_copy(out=PAD, in_=smi[:, 0, :]), [smi], [sm])
            k.op("dve", lambda e: e.memset(ONES, 1.0), [], [sm], [])
            k.op("dve", lambda e: e.tensor_tensor_scan(out=ENDS, data0=ONES, data1=PAD, initial=0.0, op0=ALU.mult, op1=ALU.add), [sm], [sm], [])
            k.op("dve", lambda e: e.tensor_tensor(out=START, in0=ENDS, in1=PAD, op=ALU.subtract), [sm], [sm], [])
            k.op("dve", lambda e: e.tensor_tensor(out=RK[:], in0=RK[:], in1=PRE[:, 0:NTT, :], op=ALU.add), [RK, PRE], [RK], [])
            k.op("dve", lambda e: e.tensor_tensor(out=RK[:], in0=RK[:], in1=sm[:, 2:3, :].to_broadcast([128, NTT, 32]), op=ALU.add),
                 [RK, sm], [RK], [])
            EQ = k.sbuf("EQ", [128, NTT, 32], F32)
            DK = k.sbuf("DK", [128, NTT, 4], F32)
            for kk in range(4):
                k.op("dve", lambda e: e.tensor_tensor(out=EQ[:], in0=LG[:], in1=mx8[:, :, kk:kk + 1].to_broadcast([128, NTT, 32]),
                                                      op=ALU.is_equal), [LG, mx8], [EQ])
                k.op("dve", lambda e: e.tensor_tensor(out=EQ[:], in0=EQ[:], in1=RK[:], op=ALU.mult), [EQ, RK], [EQ], [])
                k.op("dve", lambda e: e.tensor_reduce(out=DK[:, :, kk], in_=EQ[:], axis=AX.X, op=ALU.add), [EQ], [DK], [DK] if kk == 0 else [])
            k.op("dve", lambda e: e.tensor_copy(out=DKi[:], in_=DK[:]), [DK], [DKi])
            k.dma("sp", DESTD.rearrange("(n p) c -> p n c", p=128), DKi[:], rbuf=DKi)
            PR = k.sbuf("PR", [128, NTT, 4], F32)
            PS_ = k.sbuf("PS_", [128, 2, NTT], F32)
            k.op("dve", lambda e: e.tensor_tensor(out=PR[:], in0=mx8[:, :, 0:4], in1=mx8[:, :, 0:1].to_broadcast([128, NTT, 4]), op=ALU.subtract),
                 [mx8], [PR])
            k.op("act", lambda e: e.activation(out=PR[:], in_=PR[:], func=AF.Exp), [PR], [PR], [])
            k.op("dve", lambda e: e.tensor_reduce(out=PS_[:, 0, :], in_=PR[:], axis=AX.X, op=ALU.add), [PR], [PS_])
            k.op("dve", lambda e: e.reciprocal(out=PS_[:, 1, :], in_=PS_[:, 0, :]), [PS_], [PS_], [])
            k.op("dve", lambda e: e.tensor_tensor(out=PR[:], in0=PR[:], in1=PS_[:, 1, :].unsqueeze(2).to_broadcast([128, NTT, 4]), op=ALU.mult),
                 [PR, PS_], [PR], [])
            k.dma("sp", PROBD.rearrange("(n p) c -> p n c", p=128), PR[:], rbuf=PR)
            IOi = k.sbuf("IOi", [128, NB + 16], I32)
            k.op("pool", lambda e: e.iota(IOi[:, 0:NB], pattern=[[BLK, NB]], base=0, channel_multiplier=0), [], [IOi])
            k.op("pool", lambda e: e.iota(IOi[:, NB:NB + 8], pattern=[[128, 8]], base=0, channel_multiplier=1), [], [IOi], [])
            IOf = k.sbuf("IOf", [128, NB + 16], F32)
            k.op("dve", lambda e: e.tensor_copy(out=IOf[:, 0:NB + 8], in_=IOi[:, 0:NB + 8]), [IOi], [IOf])
            BE = k.sbuf("BE", [128, 3, NB], F32)
            k.op("dve", lambda e: e.tensor_scalar(out=BE[:, 0, :], in0=IOf[:, 0:NB], scalar1=sm[:, 1, 0:1], scalar2=None, op0=ALU.is_ge), [IOf, sm], [BE])
            for ee in range(1, 32):
                k.op("dve", lambda e: e.scalar_tensor_tensor(out=BE[:, 0, :], in0=IOf[:, 0:NB], scalar=sm[:, 1, ee:ee + 1], in1=BE[:, 0, :],
                                                             op0=ALU.is_ge, op1=ALU.add), [IOf, sm, BE], [BE], [])
            k.op("dve", lambda e: e.tensor_scalar(out=BE[:, 0, :], in0=BE[:, 0, :], scalar1=31.0, scalar2=float(l * E), op0=ALU.min, op1=ALU.add),
                 [BE], [BE], [])
            k.op("dve", lambda e: e.tensor_copy(out=BDI[:], in_=BE[:, 0, :]), [BE], [BDI])
            k.op("dve", lambda e: e.tensor_scalar(out=BE[:, 1, :], in0=BE[:, 0, :], scalar1=128.0, scalar2=IOf[:, NB:NB + 1], op0=ALU.mult, op1=ALU.add),
                 [BE, IOf], [BE], [])
            k.op("dve", lambda e: e.tensor_copy(out=BGI[:], in_=BE[:, 1, :]), [BE], [BGI])
            GUf = k.sbuf("GUf", [128, NB, 8], F32)
            for c in range(8):
                k.op("dve", lambda e: e.tensor_scalar(out=GUf[:, :, c], in0=BE[:, 0, :], scalar1=1024.0, scalar2=IOf[:, NB + c:NB + c + 1],
                                                      op0=ALU.mult, op1=ALU.add), [BE, IOf], [GUf], [GUf] if c == 0 else [])
            k.op("dve", lambda e: e.tensor_copy(out=GUI[:], in_=GUf[:]), [GUf], [GUI])
            if "BED" in dbg:
                k.dma("sp", dbgT["BED"], BDI[0:1, :], rbuf=BDI)

        def dispatch(DKi):
            r_xb = Ring(k, "dxb", 3, [128, 1024], BF16)
            for n in range(NTT):
                xb = r_xb.next()
                k.dma("pool", xb[:], X1B[n * 128:(n + 1) * 128, :], wbuf=xb)
                for kk in range(4):
                    k.dma("pool", None, None, rbuf=xb, extra_reads=[DKi],
                          fn=lambda e: e.indirect_dma_start(out=XS, out_offset=bass.IndirectOffsetOnAxis(ap=DKi[:, n, kk:kk + 1], axis=0),
                                                            in_=xb[:, :], in_offset=None))

        def experts(l, GUI, BGI, BDI):
            r_wgu = Ring(k, "wgu", 2, [128, 8, 2048], BF16)
            r_wdn = Ring(k, "wdn", 2, [128, 8, 1024], BF16)
            r_bgu = Ring(k, "bgu", 2, [128, 32], F32)
            r_bdn = Ring(k, "bdn", 2, [128, 1024], F32)
            r_xs = Ring(k, "exs", 2, [128, NSUB, 1024], BF16)
            r_xsT = Ring(k, "exsT", 1, [128, 8, BLK], BF16)
            r_act = Ring(k, "eact", 2, [128, 8, BLK], BF16)
            r_g = Ring(k, "eg", 2, [128, BLK], F32)
            r_s = Ring(k, "es", 2, [128, BLK], F32)
            r_u = Ring(k, "eu", 2, [128, BLK], F32)
            r_y = Ring(k, "ey", 2, [128, 1024], F32)
            def load_block(b):
                wgu, wdn, bgu, bdn = r_wgu.next(), r_wdn.next(), r_bgu.next(), r_bdn.next()
                for c in range(8):
                    k.dma("pool", None, None, wbuf=wgu, fresh=(c == 0), extra_reads=[GUI],
                          fn=lambda e: e.indirect_dma_start(out=wgu[:, c, :], out_offset=None, in_=w_gu,
                                                            in_offset=bass.IndirectOffsetOnAxis(ap=GUI[:, b, c:c + 1], axis=0)))
                    k.dma("pool", None, None, wbuf=wdn, fresh=(c == 0), extra_reads=[GUI],
                          fn=lambda e: e.indirect_dma_start(out=wdn[:, c, :], out_offset=None, in_=w_dn,
                                                            in_offset=bass.IndirectOffsetOnAxis(ap=GUI[:, b, c:c + 1], axis=0)))
                k.dma("pool", None, None, wbuf=bgu, extra_reads=[BGI],
                      fn=lambda e: e.indirect_dma_start(out=bgu[:, 0:16], out_offset=None, in_=b_gu,
                                                        in_offset=bass.IndirectOffsetOnAxis(ap=BGI[:, b:b + 1], axis=0)))
                k.dma("pool", None, None, wbuf=bdn, extra_reads=[BDI],
                      fn=lambda e: e.indirect_dma_start(out=bdn[:, :], out_offset=None, in_=b_dn,
                                                        in_offset=bass.IndirectOffsetOnAxis(ap=BDI[:, b:b + 1], axis=0)))
                xs = r_xs.next()
                k.dma("sp", xs[:], XS[b * BLK:(b + 1) * BLK, :].rearrange("(s p) f -> p s f", p=128), wbuf=xs)
                return wgu, wdn, bgu, bdn, xs

            def compute_block(b, bufs):
                wgu, wdn, bgu, bdn, xs = bufs
                bg1 = r_bg1.next()
                k.op("dve", lambda e: e.tensor_scalar(out=bg1[:, 0:8], in0=bgu[:, 8:16], scalar1=1.0, scalar2=None, op0=ALU.add), [bgu], [bg1])
                xsT = r_xsT.next()
                for s in range(NSUB):
                    pt = nps()
                    ptb = pt[:].bitcast(BF16)
                    for c in range(8):
                        k.op("pe", lambda e: e.transpose(out=ptb[:, c * 128:(c + 1) * 128], in_=xs[:, s, c * 128:(c + 1) * 128], identity=identb[:]),
                             [xs, identb], [pt], [pt] if c == 0 else [], inc=(c == 7))
                    copy(evq(), xsT[:, :, s * 128:(s + 1) * 128], ptb.rearrange("p (c t) -> p c t", c=8), [pt], [xsT], [xsT] if s == 0 else [])
                act = r_act.next()
                for j in range(8):
                    pgs = []
                    for jj in (j, 8 + j):
                        pm = nps()
                        for c in range(8):
                            k.op("pe", lambda e: e.matmul(pm[:, 0:BLK], lhsT=wgu[:, c, jj * 128:(jj + 1) * 128], rhs=xsT[:, c, :],
                                                          start=(c == 0), stop=(c == 7)), [wgu, xsT], [pm], [pm] if c == 0 else [], inc=(c == 7))
                        pgs.append(pm)
                    gt, st_, ut = r_g.next(), r_s.next(), r_u.next()
                    k.op("dve", lambda e: e.tensor_scalar(out=gt[:], in0=pgs[0][:, 0:BLK], scalar1=bgu[:, j:j + 1], scalar2=7.0, op0=ALU.add, op1=ALU.min),
                         [pgs[0], bgu], [gt])
                    k.op("act", lambda e: e.activation(out=st_[:], in_=gt[:], func=AF.Sigmoid, scale=1.702), [gt], [st_])
                    k.op("dve", lambda e: e.tensor_scalar(out=ut[:], in0=pgs[1][:, 0:BLK], scalar1=bg1[:, j:j + 1], scalar2=8.0, op0=ALU.add, op1=ALU.min),
                         [pgs[1], bg1], [ut])
                    k.op("dve", lambda e: e.scalar_tensor_tensor(out=ut[:], in0=ut[:], scalar=-6.0, in1=gt[:], op0=ALU.max, op1=ALU.mult), [ut, gt], [ut], [])
                    k.op("dve", lambda e: e.tensor_tensor(out=act[:, j, :], in0=ut[:], in1=st_[:], op=ALU.mult), [ut, st_], [act], [act] if j == 0 else [])
                for s in range(NSUB):
                    yo = r_y.next()
                    for half in range(2):
                        pd = nps()
                        for c in range(8):
                            k.op("pe", lambda e: e.matmul(pd[:], lhsT=act[:, c, s * 128:(s + 1) * 128], rhs=wdn[:, c, half * 512:(half + 1) * 512],
                                                          start=(c == 0), stop=(c == 7)), [act, wdn], [pd], [pd] if c == 0 else [], inc=(c == 7))
                        k.op("dve", lambda e: e.tensor_tensor(out=yo[:, half * 512:(half + 1) * 512], in0=pd[:], in1=bdn[:, half * 512:(half + 1) * 512],
                                                              op=ALU.add), [pd, bdn], [yo], [yo] if half == 0 else [])
                    r0 = b * BLK + s * 128
                    for half in range(2):
                        k.dma("sp", YSH[half][r0:r0 + 128, :], yo[:, half * 512:(half + 1) * 512], rbuf=yo)

            r_bg1 = Ring(k, "bg1", 2, [128, 8], F32)
            pend = load_block(0)
            for b in range(NB):
                nxt = load_block(b + 1) if b + 1 < NB else None
                compute_block(b, pend)
                pend = nxt

        def phase4(l, dst):
            wpg = k.sbuf("wpg", [128, 8, 1024], BF16)
            for c in range(8):
                k.dma("pool", wpg[:, c, :], w_pg[l, c * 128:(c + 1) * 128, :], wbuf=wpg, fresh=(c == 0))
            wpp = k.sbuf("wpp", [128, 2, 1024], BF16)
            k.dma("pool", wpp[:], w_pp[l].rearrange("(c p) n -> p c n", p=128), wbuf=wpp)
            lg_, lb_ = k.sbuf("ln2g", [128, 1024], F32), k.sbuf("ln2b", [128, 1024], F32)
            k.dma("sp", lg_[:], ln2g[l], wbuf=lg_)
            k.dma("sp", lb_[:], ln2b[l], wbuf=lb_)
            DKi = k.sbuf("DKi4", [128, NTT, 4], I32)
            k.dma("sp", DKi[:], DESTD.rearrange("(n p) c -> p n c", p=128), wbuf=DKi)
            PR = k.sbuf("PR4", [128, NTT, 4], F32)
            k.dma("sp", PR[:], PROBD.rearrange("(n p) c -> p n c", p=128), wbuf=PR)
            r_x1 = Ring(k, "fx1", 2, [128, 1024], F32)
            r_xT = Ring(k, "fxT", 2, [128, 8, 128], BF16)
            r_pb = Ring(k, "fpb", 2, [128, 256], BF16)
            r_pf = Ring(k, "fpf", 2, [128, 256], F32)
            r_pT = Ring(k, "fpT", 2, [128, 2, 128], BF16)
            r_yk = Ring(k, "fyk", 2, [128, 4, 1024], F32)
            r_sg = Ring(k, "fsg", 2, [128, 1024], F32)
            r_acc = Ring(k, "facc", 2, [128, 1024], F32)
            r_o = Ring(k, "fo", 2, [128, 1024], F32)
            r_st = Ring(k, "fst", 4, [128, 32], F32)
            def load4(n):
                r0 = n * 128
                x1 = r_x1.next()
                k.dma("sp", x1[:], X1[r0:r0 + 128, :], wbuf=x1)
                xT = r_xT.next()
                k.dma("act", xT[:], X1T[:, :, r0:r0 + 128].rearrange("c p t -> p c t"), wbuf=xT)
                pf = r_pf.next()
                k.dma("act", pf[:], p_in[l, r0:r0 + 128, :], wbuf=pf)
                pb = pf
                yk = r_yk.next()
                for kk in range(4):
                    for half in range(2):
                        k.dma("pool", None, None, wbuf=yk, fresh=(kk == 0 and half == 0), extra_reads=[DKi],
                              fn=lambda e: e.indirect_dma_start(out=yk[:, kk, half * 512:(half + 1) * 512], out_offset=None, in_=YSH[half],
                                                                in_offset=bass.IndirectOffsetOnAxis(ap=DKi[:, n, kk:kk + 1], axis=0)))
                return x1, xT, pb, yk

            pend4 = load4(0)
            for n in range(NTT):
                r0 = n * 128
                x1, xT, pb, yk = pend4
                if n + 1 < NTT:
                    pend4 = load4(n + 1)
                pf = pb
                pb = r_pb.next()
                copy("act", pb[:], pf[:], [pf], [pb])
                pt = nps()
                ptb = pt[:].bitcast(BF16)
                for c in range(2):
                    k.op("pe", lambda e: e.transpose(out=ptb[:, c * 128:(c + 1) * 128], in_=pb[:, c * 128:(c + 1) * 128], identity=identb[:]),
                         [pb, identb], [pt], [pt] if c == 0 else [], inc=(c == 1))
                pT = r_pT.next()
                copy("act", pT[:], ptb[:, 0:256].rearrange("p (c t) -> p c t", c=2), [pt], [pT])
                sg, acc = r_sg.next(), r_acc.next()
                for half in range(2):
                    hs = slice(half * 512, (half + 1) * 512)
                    pg = nps()
                    for c in range(8):
                        k.op("pe", lambda e: e.matmul(pg[:], lhsT=xT[:, c, :], rhs=wpg[:, c, hs], start=(c == 0), stop=(c == 7)),
                             [xT, wpg], [pg], [pg] if c == 0 else [], inc=(c == 7))
                    k.op("act", lambda e: e.activation(out=sg[:, hs], in_=pg[:], func=AF.Sigmoid), [pg], [sg], [sg] if half == 0 else [])
                    pp = nps()
                    for c in range(2):
                        k.op("pe", lambda e: e.matmul(pp[:], lhsT=pT[:, c, :], rhs=wpp[:, c, hs], start=(c == 0), stop=(c == 1)),
                             [pT, wpp], [pp], [pp] if c == 0 else [], inc=(c == 1))
                    k.op("dve", lambda e: e.tensor_tensor(out=acc[:, hs], in0=pp[:], in1=sg[:, hs], op=ALU.mult), [pp, sg], [acc], [acc] if half == 0 else [])
                k.op("dve", lambda e: e.scalar_tensor_tensor(out=acc[:], in0=x1[:], scalar=ALPHA, in1=acc[:], op0=ALU.mult, op1=ALU.add), [x1, acc], [acc], [])
                for kk in range(4):
                    k.op("dve", lambda e: e.scalar_tensor_tensor(out=acc[:], in0=yk[:, kk, :], scalar=PR[:, n, kk:kk + 1], in1=acc[:],
                                                                 op0=ALU.mult, op1=ALU.add), [yk, PR, acc], [acc], [])
                o = r_o.next()
                layer_norm_tm(acc, o, lg_, lb_, r_st, e1="dve", e2="pool")
                k.dma("sp", dst[r0:r0 + 128, :], o[:], rbuf=o)


        k1_eps = k.sbuf("k1_eps", [128, 1], F32)
        k.op("dve", lambda g: g.memset(k1_eps[:], LN_EPS), [], [k1_eps])

        for l in range(NL):
            xsrc = x_in if l == 0 else XN
            if "p1" in phases:
                with k.scope():
                    phase1(l, xsrc)
            if "pa" in phases:
                with k.scope():
                    phaseA(l)
            if "p2a" in phases:
                with k.scope():
                    phase2a(l, xsrc)
            if "p2b" in phases:
                with k.scope():
                    phase2b(l, xsrc)
            if "p3" in phases:
                with k.scope():
                    phase3(l)
            if "p4" in phases:
                with k.scope():
                    phase4(l, y_out if l == NL - 1 else XN)
        k.barrier()
    return nc


_last_raw = None


def random_weights(rng, NL=2):
    f = np.float32

    def nrm(shape, s):
        return (rng.standard_normal(shape) * s).astype(f)
    beta = 16.0 ** -0.25
    return {
        'w_in': nrm((NL, D, NIN), D ** -0.5), 'spatial_w': nrm((NL, 4, 128, 128), 128 ** -0.5),
        'spatial_b': 1.0 + nrm((NL, 4, 128), 0.02), 'conv_w': nrm((NL, 3, 512), 3 ** -0.5),
        'pool_w': nrm((NL, 4, 128, 128), 128 ** -0.5), 'pool_scale': 1.0 + nrm((NL, 512), 0.02),
        'w_branch': nrm((NL, 4, 512, D), 512 ** -0.5), 'w_gate': nrm((NL, 4, D, D), D ** -0.5),
        'w_out': nrm((NL, D, D), beta * D ** -0.5), 'ln1_g': 1.0 + nrm((NL, D), 0.02), 'ln1_b': nrm((NL, D), 0.02),
        'router_w': nrm((NL, D, E), D ** -0.5), 'router_b': nrm((NL, E), 0.01),
        'w_gate_up': nrm((NL, E, D, 2 * FF), D ** -0.5), 'b_gate_up': nrm((NL, E, 2 * FF), 0.02),
        'w_down': nrm((NL, E, FF, D), beta * FF ** -0.5), 'b_down': nrm((NL, E, D), 0.02),
        'w_ple_gate': nrm((NL, D, D), D ** -0.5), 'w_ple_proj': nrm((NL, PLE, D), beta * PLE ** -0.5),
        'ln2_g': 1.0 + nrm((NL, D), 0.02), 'ln2_b': nrm((NL, D), 0.02),
    }


def make_weights_inputs(W, NL=2):
    global _last_raw
    _last_raw = W
    f = np.float32
    c = np.ascontiguousarray

    def rep(a):
        a = np.asarray(a, f)
        return c(np.broadcast_to(a[:, None, :], (a.shape[0], 128, a.shape[1])))
    out = {
        "w_in": c(np.asarray(W['w_in'], f)),
        "spT": c(np.asarray(W['spatial_w'], f).transpose(0, 3, 1, 2)),
        "sbias": c(np.broadcast_to(np.asarray(W['spatial_b'], f)[:, None], (NL, 128, 4, 128))),
        "convw": c(np.asarray(W['conv_w'], f).reshape(NL, 3, 4, 128).transpose(0, 3, 1, 2)),
        "poolw": c(np.asarray(W['pool_w'], f).transpose(0, 2, 1, 3)),
        "pscale": c(np.asarray(W['pool_scale'], f).reshape(NL, 4, 128).transpose(0, 2, 1)),
        "w_branch": c(np.asarray(W['w_branch'], f)), "w_gate": c(np.asarray(W['w_gate'], f)),
        "w_out": c(np.asarray(W['w_out'], f)),
        "ln1g": rep(W['ln1_g']), "ln1b": rep(W['ln1_b']),
        "router_w": c(np.asarray(W['router_w'], f)), "router_b": rep(W['router_b']),
        "w_gu": np.asarray(W['w_gate_up'], f).reshape(NL * E * D, 2 * FF),
        "b_gu": c(np.asarray(W['b_gate_up'], f).reshape(NL, E, 16, 128).transpose(0, 1, 3, 2)).reshape(NL * E * 128, 16),
        "w_dn": np.asarray(W['w_down'], f).reshape(NL * E * FF, D),
        "b_dn": np.asarray(W['b_down'], f).reshape(NL * E, D),
        "w_pg": c(np.asarray(W['w_ple_gate'], f)), "w_pp": c(np.asarray(W['w_ple_proj'], f)),
        "ln2g": rep(W['ln2_g']), "ln2b": rep(W['ln2_b']),
        "identf": np.eye(128, dtype=f),
        "abias": att_bias_tables().reshape(128, -1),
        "pcorr": pool_corr().reshape(128, -1),
        "tril": np.triu(np.ones((128, 128), f), 1),
    }
    return out


def kernel(x_prompt, x_sample, p_prompt, p_sample, w_in, spatial_w, spatial_b, conv_w, pool_w, pool_scale,
           w_branch, w_gate, w_out, ln1_g, ln1_b, router_w, router_b, w_gate_up, b_gate_up, w_down, b_down,
           w_ple_gate, w_ple_proj, ln2_g, ln2_b):
    NCORE = 8
    f = np.float32
    x_prompt, x_sample = np.asarray(x_prompt, f), np.asarray(x_sample, f)
    p_prompt, p_sample = np.asarray(p_prompt, f), np.asarray(p_sample, f)
    NL = p_prompt.shape[0]
    B, S, _ = x_prompt.shape
    DB, DS, _ = x_sample.shape
    per = DB // NCORE
    seqs = [S] + [DS] * per
    W = dict(w_in=w_in, spatial_w=spatial_w, spatial_b=spatial_b, conv_w=conv_w, pool_w=pool_w, pool_scale=pool_scale,
             w_branch=w_branch, w_gate=w_gate, w_out=w_out, ln1_g=ln1_g, ln1_b=ln1_b, router_w=router_w, router_b=router_b,
             w_gate_up=w_gate_up, b_gate_up=b_gate_up, w_down=w_down, b_down=b_down, w_ple_gate=w_ple_gate,
             w_ple_proj=w_ple_proj, ln2_g=ln2_g, ln2_b=ln2_b)
    shared = make_weights_inputs(W, NL=NL)
    in_maps = []
    for c in range(NCORE):
        m = dict(shared)
        m["x"] = np.ascontiguousarray(np.concatenate([x_prompt[c], x_sample[c * per:(c + 1) * per].reshape(per * DS, D)], 0))
        m["p"] = np.ascontiguousarray(np.concatenate([p_prompt[:, c], p_sample[:, c * per:(c + 1) * per].reshape(NL, per * DS, PLE)], 1))
        in_maps.append(m)
    nc = build(seqs, NL=NL)
    res = run_bass_kernel_spmd(nc, in_maps, core_ids=list(range(NCORE)))
    y_prompt = np.empty((B, S, D), f)
    y_sample = np.empty((DB, DS, D), f)
    for c in range(NCORE):
        y = np.asarray(res.results[c]["y"])
        y_prompt[c] = y[:S]
        y_sample[c * per:(c + 1) * per] = y[S:].reshape(per, DS, D)
    return (y_prompt, y_sample)
```
